# Optimizing a Trainium2 kernel written in Bass

```python
import math
import jax, jax.numpy as jnp
from jax import lax
import numpy as np

D_MODEL = 1024
BATCH = 16
SEQ = 2048
DEPTH = 1

N_Q_HEADS = 16
N_KV_HEADS = 2
HEAD_DIM = 64
WINDOW = 128
ROPE_THETA = 10000.0
Q_WIDTH = N_Q_HEADS * HEAD_DIM
KV_WIDTH = N_KV_HEADS * HEAD_DIM
SSM_GROUP = 16
SSM_GROUPS = 32
SSM_WIDTH = SSM_GROUP * SSM_GROUPS
SSM_STATE = 64
DT_MIN = 0.001
DT_MAX = 0.1
IN_WIDTH = Q_WIDTH + 2 * KV_WIDTH + SSM_WIDTH + 2 * D_MODEL
N_MEM = 256
N_CROSS_HEADS = 4
CROSS_HEAD_DIM = D_MODEL // N_CROSS_HEADS
N_EXPERTS = 32
TOP_K = 4
D_EXPERT = D_MODEL
SWIGLU_ALPHA = 1.702
SWIGLU_LIMIT = 7.0
MOE_BLOCK = 256
LN_EPS = 1e-5
DEEPNORM_ALPHA = (2 * DEPTH) ** 0.25
DEEPNORM_BETA = (8 * DEPTH) ** -0.25

kernel_name = "hybrid_swa_s5_moe_deepnorm"


def layer_norm(x, g, b):
    xf = x.astype(jnp.float32)
    mu = jnp.mean(xf, axis=-1, keepdims=True)
    var = jnp.mean(jnp.square(xf - mu), axis=-1, keepdims=True)
    return ((xf - mu) * lax.rsqrt(var + LN_EPS)).astype(x.dtype) * g + b


def rope(t, positions):
    half = HEAD_DIM // 2
    inv_freq = jnp.power(ROPE_THETA, -jnp.arange(half, dtype=jnp.float32) / half)
    ang = positions.astype(jnp.float32)[..., None] * inv_freq
    cos = jnp.cos(ang)[:, :, None, :]
    sin = jnp.sin(ang)[:, :, None, :]
    t1 = t[..., :half].astype(jnp.float32)
    t2 = t[..., half:].astype(jnp.float32)
    out = jnp.concatenate([t1 * cos - t2 * sin, t2 * cos + t1 * sin], axis=-1)
    return out.astype(t.dtype)


def sliding_window_gqa(q, k, v, sinks):
    bsz, seq = q.shape[0], q.shape[1]
    nb = seq // WINDOW
    rep = N_Q_HEADS // N_KV_HEADS
    qb = q.reshape(bsz, nb, WINDOW, N_KV_HEADS, rep, HEAD_DIM)

    def band(t):
        tb = t.reshape(bsz, nb, WINDOW, N_KV_HEADS, HEAD_DIM)
        prev = jnp.pad(tb, ((0, 0), (1, 0), (0, 0), (0, 0), (0, 0)))[:, :-1]
        return jnp.concatenate([prev, tb], axis=2)

    kb, vb = band(k), band(v)
    scores = jnp.einsum('bnqkrd,bnckd->bnkrqc', qb, kb).astype(jnp.float32) * (HEAD_DIM ** -0.5)
    qi = jnp.arange(WINDOW)[:, None]
    ci = jnp.arange(2 * WINDOW)[None, :]
    diff = qi + WINDOW - ci
    local = (diff >= 0) & (diff < WINDOW)
    kpos = jnp.arange(nb)[:, None] * WINDOW + ci - WINDOW
    mask = local[None] & (kpos >= 0)[:, None, :]
    scores = jnp.where(mask[None, :, None, None], scores, -jnp.inf)
    sink = sinks.astype(jnp.float32).reshape(N_KV_HEADS, rep)[None, None, :, :, None, None]
    m = jnp.maximum(jnp.max(scores, axis=-1, keepdims=True), sink)
    p = jnp.exp(scores - m)
    w = p / (jnp.sum(p, axis=-1, keepdims=True) + jnp.exp(sink - m))
    out = jnp.einsum('bnkrqc,bnckd->bnqkrd', w.astype(v.dtype), vb)
    return out.reshape(bsz, seq, Q_WIDTH)


def s5_ssm(u, lam_re, lam_im, log_dt, b_re, b_im, c_re, c_im, d_skip):
    bsz, seq = u.shape[0], u.shape[1]
    uf = u.astype(jnp.float32).reshape(bsz, seq, SSM_GROUPS, SSM_GROUP)
    lr = lam_re.astype(jnp.float32)
    li = lam_im.astype(jnp.float32)
    dt = jnp.exp(log_dt.astype(jnp.float32))[:, None]
    mag = jnp.exp(lr * dt)
    abar_re = mag * jnp.cos(li * dt)
    abar_im = mag * jnp.sin(li * dt)
    den = lr * lr + li * li
    f_re = ((abar_re - 1.0) * lr + abar_im * li) / den
    f_im = (abar_im * lr - (abar_re - 1.0) * li) / den
    br = b_re.astype(jnp.float32)
    bi = b_im.astype(jnp.float32)
    bbar_re = f_re[..., None] * br - f_im[..., None] * bi
    bbar_im = f_re[..., None] * bi + f_im[..., None] * br
    bu_re = jnp.einsum('bsgh,gph->bsgp', uf, bbar_re)
    bu_im = jnp.einsum('bsgh,gph->bsgp', uf, bbar_im)
    a_re = jnp.broadcast_to(abar_re, (1, seq, SSM_GROUPS, SSM_STATE))
    a_im = jnp.broadcast_to(abar_im, (1, seq, SSM_GROUPS, SSM_STATE))

    def combine(e1, e2):
        a1r, a1i, b1r, b1i = e1
        a2r, a2i, b2r, b2i = e2
        return (a2r * a1r - a2i * a1i,
                a2r * a1i + a2i * a1r,
                a2r * b1r - a2i * b1i + b2r,
                a2r * b1i + a2i * b1r + b2i)

    _, _, h_re, h_im = lax.associative_scan(combine, (a_re, a_im, bu_re, bu_im), axis=1)
    y = (jnp.einsum('bsgp,ghp->bsgh', h_re, c_re.astype(jnp.float32))
         - jnp.einsum('bsgp,ghp->bsgh', h_im, c_im.astype(jnp.float32)))
    y = y + d_skip.astype(jnp.float32).reshape(SSM_GROUPS, SSM_GROUP) * uf
    return y.reshape(bsz, seq, SSM_WIDTH).astype(u.dtype)


def memory_cross_attention(x, mem, wq, wk, wv, wo):
    bsz, seq = x.shape[0], x.shape[1]
    n_mem = mem.shape[1]
    q = (x @ wq).reshape(bsz, seq, N_CROSS_HEADS, CROSS_HEAD_DIM)
    k = (mem @ wk).reshape(bsz, n_mem, N_CROSS_HEADS, CROSS_HEAD_DIM)
    v = (mem @ wv).reshape(bsz, n_mem, N_CROSS_HEADS, CROSS_HEAD_DIM)
    s = jnp.einsum('bshd,bmhd->bhsm', q, k).astype(jnp.float32) * (CROSS_HEAD_DIM ** -0.5)
    w = jax.nn.softmax(s, axis=-1).astype(x.dtype)
    o = jnp.einsum('bhsm,bmhd->bshd', w, v).reshape(bsz, seq, D_MODEL)
    return o @ wo


def clamped_swiglu(gu):
    g = jnp.minimum(gu[..., ::2], SWIGLU_LIMIT)
    lin = jnp.clip(gu[..., 1::2], -SWIGLU_LIMIT, SWIGLU_LIMIT)
    return g * jax.nn.sigmoid(SWIGLU_ALPHA * g) * (lin + 1.0)


def moe_ffn(h, w_router, b_router, w_gate_up, b_gate_up, w_down, b_down):
    n_tok = h.shape[0]
    logits = (h @ w_router + b_router).astype(jnp.float32)
    top_val, top_idx = lax.top_k(logits, TOP_K)
    gates = jax.nn.softmax(top_val, axis=-1).astype(h.dtype)
    n_assign = n_tok * TOP_K
    flat_e = top_idx.reshape(-1)
    flat_tok = jnp.repeat(jnp.arange(n_tok, dtype=jnp.int32), TOP_K)
    flat_gate = gates.reshape(-1)
    order = jnp.argsort(flat_e)
    sorted_e = flat_e[order]
    counts = jnp.bincount(flat_e, length=N_EXPERTS)
    padded = (counts + MOE_BLOCK - 1) // MOE_BLOCK * MOE_BLOCK
    ends_pad = jnp.cumsum(padded)
    start_pad = ends_pad - padded
    start = jnp.cumsum(counts) - counts
    dest = start_pad[sorted_e] + jnp.arange(n_assign, dtype=jnp.int32) - start[sorted_e]
    n_blocks = -(-n_assign // MOE_BLOCK) + N_EXPERTS
    n_slots = n_blocks * MOE_BLOCK
    slot_tok = jnp.zeros((n_slots,), jnp.int32).at[dest].set(flat_tok[order])
    slot_gate = jnp.zeros((n_slots,), h.dtype).at[dest].set(flat_gate[order])
    block_start = jnp.arange(n_blocks, dtype=ends_pad.dtype) * MOE_BLOCK
    block_expert = jnp.minimum(jnp.searchsorted(ends_pad, block_start, side='right'), N_EXPERTS - 1)
    xb = h[slot_tok].reshape(n_blocks, MOE_BLOCK, h.shape[1])

    def run_block(args):
        xblk, e = args
        gu = xblk @ w_gate_up[e] + b_gate_up[e]
        return clamped_swiglu(gu) @ w_down[e] + b_down[e]

    yb = lax.map(run_block, (xb, block_expert))
    y = yb.reshape(n_slots, h.shape[1]) * slot_gate[:, None]
    return jax.ops.segment_sum(y, slot_tok, num_segments=n_tok)


def setup_inputs(seed: int = 0) -> dict:
    key = jax.random.key(seed)
    ks = iter(jax.random.split(key, 40))
    L = DEPTH

    def nrm(shape, scale):
        return jax.random.normal(next(ks), shape, jnp.float32) * scale

    x = nrm((BATCH, SEQ, D_MODEL), 1.0)
    mem = nrm((BATCH, N_MEM, D_MODEL), 1.0)
    offset = jax.random.randint(next(ks), (BATCH, 1), 0, 1024, dtype=jnp.int32)
    positions = (jnp.arange(SEQ, dtype=jnp.int32)[None, :] + offset).astype(jnp.int32)

    w_in = nrm((L, D_MODEL, IN_WIDTH), D_MODEL ** -0.5)
    sinks = nrm((L, N_Q_HEADS), 0.5)
    w_attn_o = nrm((L, Q_WIDTH, D_MODEL), Q_WIDTH ** -0.5)
    lam_re = -0.5 + nrm((L, SSM_GROUPS, SSM_STATE), 0.01)
    lam_im = (math.pi * jnp.arange(SSM_STATE, dtype=jnp.float32))[None, None, :] + nrm((L, SSM_GROUPS, SSM_STATE), 0.01)
    log_dt = jax.random.uniform(next(ks), (L, SSM_GROUPS), jnp.float32, math.log(DT_MIN), math.log(DT_MAX))
    b_re = nrm((L, SSM_GROUPS, SSM_STATE, SSM_GROUP), (2 * SSM_GROUP) ** -0.5)
    b_im = nrm((L, SSM_GROUPS, SSM_STATE, SSM_GROUP), (2 * SSM_GROUP) ** -0.5)
    c_re = nrm((L, SSM_GROUPS, SSM_GROUP, SSM_STATE), (2 * SSM_STATE) ** -0.5)
    c_im = nrm((L, SSM_GROUPS, SSM_GROUP, SSM_STATE), (2 * SSM_STATE) ** -0.5)
    d_skip = nrm((L, SSM_WIDTH), 1.0)
    w_glu_a = nrm((L, SSM_WIDTH, D_MODEL), SSM_WIDTH ** -0.5)
    w_glu_b = nrm((L, SSM_WIDTH, D_MODEL), SSM_WIDTH ** -0.5)
    w_out = nrm((L, D_MODEL, D_MODEL), D_MODEL ** -0.5 * DEEPNORM_BETA)
    ln1_g = 1.0 + nrm((L, D_MODEL), 0.05)
    ln1_b = nrm((L, D_MODEL), 0.02)

    wq_c = nrm((L, D_MODEL, D_MODEL), D_MODEL ** -0.5)
    wk_c = nrm((L, D_MODEL, D_MODEL), D_MODEL ** -0.5)
    wv_c = nrm((L, D_MODEL, D_MODEL), D_MODEL ** -0.5)
    wo_c = nrm((L, D_MODEL, D_MODEL), D_MODEL ** -0.5 * DEEPNORM_BETA)
    ln2_g = 1.0 + nrm((L, D_MODEL), 0.05)
    ln2_b = nrm((L, D_MODEL), 0.02)

    w_router = nrm((L, D_MODEL, N_EXPERTS), D_MODEL ** -0.5)
    b_router = nrm((L, N_EXPERTS), 0.01)
    w_gate_up = nrm((L, N_EXPERTS, D_MODEL, 2 * D_EXPERT), D_MODEL ** -0.5)
    b_gate_up = nrm((L, N_EXPERTS, 2 * D_EXPERT), 0.02)
    w_down = nrm((L, N_EXPERTS, D_EXPERT, D_MODEL), D_EXPERT ** -0.5 * DEEPNORM_BETA)
    b_down = nrm((L, N_EXPERTS, D_MODEL), 0.02)
    ln3_g = 1.0 + nrm((L, D_MODEL), 0.05)
    ln3_b = nrm((L, D_MODEL), 0.02)

    return {"x": x, "mem": mem, "positions": positions,
            "w_in": w_in, "sinks": sinks, "w_attn_o": w_attn_o,
            "lam_re": lam_re, "lam_im": lam_im, "log_dt": log_dt,
            "b_re": b_re, "b_im": b_im, "c_re": c_re, "c_im": c_im, "d_skip": d_skip,
            "w_glu_a": w_glu_a, "w_glu_b": w_glu_b, "w_out": w_out,
            "ln1_g": ln1_g, "ln1_b": ln1_b,
            "wq_c": wq_c, "wk_c": wk_c, "wv_c": wv_c, "wo_c": wo_c,
            "ln2_g": ln2_g, "ln2_b": ln2_b,
            "w_router": w_router, "b_router": b_router,
            "w_gate_up": w_gate_up, "b_gate_up": b_gate_up,
            "w_down": w_down, "b_down": b_down,
            "ln3_g": ln3_g, "ln3_b": ln3_b}


def reference(x, mem, positions, w_in, sinks, w_attn_o, lam_re, lam_im, log_dt,
              b_re, b_im, c_re, c_im, d_skip, w_glu_a, w_glu_b, w_out, ln1_g, ln1_b,
              wq_c, wk_c, wv_c, wo_c, ln2_g, ln2_b, w_router, b_router,
              w_gate_up, b_gate_up, w_down, b_down, ln3_g, ln3_b):
    bsz, seq, d = x.shape
    o_k = Q_WIDTH
    o_v = o_k + KV_WIDTH
    o_s = o_v + KV_WIDTH
    o_ga = o_s + SSM_WIDTH
    o_gs = o_ga + D_MODEL
    for l in range(DEPTH):
        proj = x @ w_in[l]
        q = rope(proj[..., :o_k].reshape(bsz, seq, N_Q_HEADS, HEAD_DIM), positions)
        k = rope(proj[..., o_k:o_v].reshape(bsz, seq, N_KV_HEADS, HEAD_DIM), positions)
        v = proj[..., o_v:o_s].reshape(bsz, seq, N_KV_HEADS, HEAD_DIM)
        u_ssm = proj[..., o_s:o_ga]
        gate_a = jax.nn.sigmoid(proj[..., o_ga:o_gs])
        gate_s = jax.nn.sigmoid(proj[..., o_gs:])

        attn_out = sliding_window_gqa(q, k, v, sinks[l]) @ w_attn_o[l]

        y_ssm = s5_ssm(u_ssm, lam_re[l], lam_im[l], log_dt[l], b_re[l], b_im[l],
                       c_re[l], c_im[l], d_skip[l])
        z = jax.nn.gelu(y_ssm)
        ssm_out = (z @ w_glu_a[l]) * jax.nn.sigmoid(z @ w_glu_b[l])

        mixed = (gate_a * attn_out + gate_s * ssm_out) @ w_out[l]
        x = layer_norm(DEEPNORM_ALPHA * x + mixed, ln1_g[l], ln1_b[l])

        cross = memory_cross_attention(x, mem, wq_c[l], wk_c[l], wv_c[l], wo_c[l])
        x = layer_norm(DEEPNORM_ALPHA * x + cross, ln2_g[l], ln2_b[l])

        ffn = moe_ffn(x.reshape(bsz * seq, d), w_router[l], b_router[l], w_gate_up[l],
                      b_gate_up[l], w_down[l], b_down[l]).reshape(bsz, seq, d)
        x = layer_norm(DEEPNORM_ALPHA * x + ffn, ln3_g[l], ln3_b[l])
    return x
```

```python
import math
import numpy as np
from contextlib import ExitStack
import concourse.bass as bass
import concourse.mybir as mybir
from concourse.bass_utils import run_bass_kernel_spmd

F32 = mybir.dt.float32
BF16 = mybir.dt.bfloat16
I32 = mybir.dt.int32
U32 = mybir.dt.uint32
AF = mybir.ActivationFunctionType
ALU = mybir.AluOpType
AX = mybir.AxisListType

PE, ACT, DVE, POOL, SP = "pe", "act", "dve", "pool", "sp"

NCORES = 8
D = 1024
SEQ = 2048
NSEQ = 2
NT = NSEQ * SEQ // 128
TOK = NSEQ * SEQ
INW = 3840
NE = 32
CAP = 768
NSLOT = NE * CAP
ALPHA = 2.0 ** 0.25
LN_EPS = 1e-5
PI = math.pi


class Buf:
    __slots__ = ("name", "last_w", "readers")

    def __init__(self, name):
        self.name = name
        self.last_w = None
        self.readers = []


class Op:
    __slots__ = ("eng", "fn", "reads", "writes", "deps", "signal", "cnt", "is_dma", "ring", "ringval")

    def __init__(self, eng, fn, reads, writes, is_dma):
        self.eng = eng
        self.fn = fn
        self.reads = reads
        self.writes = writes
        self.deps = []
        self.signal = False
        self.cnt = None
        self.is_dma = is_dma
        self.ring = None
        self.ringval = None


class _Stop(Exception):
    pass


class _Rec:
    def __init__(self):
        self.calls = []

    def __getattr__(self, name):
        def f(*args, **kwargs):
            self.calls.append((name, args, kwargs))
            return None
        return f


class TB:
    __slots__ = ("t", "b")

    def __init__(self, t, b):
        self.t = t
        self.b = b


class Prog:
    NRING = 8

    def __init__(self, nc, stack):
        self.nc = nc
        self.ops = []
        self.engs = {PE: nc.tensor, ACT: nc.scalar, DVE: nc.vector, POOL: nc.gpsimd, SP: nc.sync}
        self.sem = {e: stack.enter_context(nc.semaphore("s_" + e)) for e in (PE, ACT, DVE, POOL)}
        self.rings = {e: [stack.enter_context(nc.semaphore("r_%s%d" % (e, i))) for i in range(self.NRING)]
                      for e in (SP, POOL, ACT)}
        self.cnt = {e: 0 for e in (PE, ACT, DVE, POOL)}
        self.dma_n = {e: 0 for e in (SP, POOL, ACT)}
        self.ring_issued = {e: [0] * self.NRING for e in (SP, POOL, ACT)}
        self.known = {e: {} for e in (PE, ACT, DVE, POOL, SP)}
        self.bufs = []
        self.rr = 0

    def buf(self, name):
        b = Buf(name)
        self.bufs.append(b)
        return b

    def op(self, eng, fn, reads=(), writes=(), dma=False):
        rec = _Rec()
        fn(rec)
        assert len(rec.calls) == 1
        o = Op(eng, rec.calls[0], [r.b if isinstance(r, TB) else r for r in reads],
               [w.b if isinstance(w, TB) else w for w in writes], dma)
        self.ops.append(o)
        return o

    def dma(self, eng, out, in_, reads=(), writes=(), **kw):
        return self.op(eng, lambda e: e.dma_start(out=out, in_=in_, **kw), reads, writes, dma=True)

    def flush(self):
        ops = self.ops
        self.ops = []
        last = {}
        for o in ops:
            if not o.is_dma:
                last[o.eng] = o
        for o in last.values():
            o.signal = True
        for o in ops:
            deps = []
            for b in o.reads:
                if b.last_w is not None:
                    deps.append(b.last_w)
            for b in o.writes:
                if b.last_w is not None:
                    deps.append(b.last_w)
                deps.extend(b.readers)
            seen = set()
            for d in deps:
                if d is o or id(d) in seen:
                    continue
                seen.add(id(d))
                if d.eng == PE and o.eng == PE and not d.is_dma and not o.is_dma:
                    continue
                o.deps.append(d)
                d.signal = True
            for b in o.reads:
                b.readers.append(o)
            for b in o.writes:
                b.last_w = o
                b.readers = []
        for o in ops:
            eng = self.engs[o.eng]
            kn = self.known[o.eng]
            need = {}
            for d in o.deps:
                if d.is_dma:
                    key = ("r", d.eng, d.ring)
                    need[key] = max(need.get(key, 0), d.ringval)
                else:
                    key = ("c", d.eng)
                    need[key] = max(need.get(key, 0), d.cnt)
            if o.is_dma:
                ring = self.dma_n[o.eng] % self.NRING
                prev = self.ring_issued[o.eng][ring]
                if prev > 0:
                    key = ("r", o.eng, ring)
                    need[key] = max(need.get(key, 0), prev)
            for key, val in need.items():
                if kn.get(key, 0) >= val:
                    continue
                kn[key] = val
                if key[0] == "c":
                    eng.wait_ge(self.sem[key[1]], val)
                else:
                    eng.wait_ge(self.rings[key[1]][key[2]], val)
            name_, args_, kwargs_ = o.fn
            ins = getattr(eng, name_)(*args_, **kwargs_)
            if o.is_dma:
                n = self.dma_n[o.eng]
                ring = n % self.NRING
                self.dma_n[o.eng] = n + 1
                val = self.ring_issued[o.eng][ring] + 16
                self.ring_issued[o.eng][ring] = val
                o.ring = ring
                o.ringval = val
                ins.then_inc(self.rings[o.eng][ring], 16)
            elif o.signal:
                self.cnt[o.eng] += 1
                o.cnt = self.cnt[o.eng]
                ins.then_inc(self.sem[o.eng], 1)
            o.fn = None
        for en, eng in self.engs.items():
            kn = self.known[en]
            for ce in (PE, ACT, DVE, POOL):
                v = self.cnt[ce]
                if ce != en and v > kn.get(("c", ce), 0):
                    kn[("c", ce)] = v
                    eng.wait_ge(self.sem[ce], v)
            for qe in self.rings:
                for r in range(self.NRING):
                    v = self.ring_issued[qe][r]
                    if v > kn.get(("r", qe, r), 0):
                        kn[("r", qe, r)] = v
                        eng.wait_ge(self.rings[qe][r], v)
        for b in self.bufs:
            b.last_w = None
            b.readers = []


def build_nc(debug=False, nphase=9, dbg3=0, skip=(), dbgt=0):
    nc = bass.Bass("TRN2", target_bir_lowering=False)

    def din(name, shape, dt=F32):
        return nc.dram_tensor(name, list(shape), dt, kind="ExternalInput").ap()

    def dscr(name, shape, dt=F32):
        return nc.dram_tensor(name, list(shape), dt, kind=("ExternalOutput" if debug else "Internal")).ap()

    x_d = din("x", [TOK, D])
    mem_d = din("mem", [NSEQ * 256, D])
    pos_d = din("pos_t", [128, NT], I32)
    w_in_d = din("w_in", [D, INW])
    sinks_d = din("sinks", [1, 16])
    w_attn_o_d = din("w_attn_o", [D, D])
    lr_d = din("lamre_t", [128, 32])
    li_d = din("lamim_t", [128, 32])
    ldt_d = din("logdt_t", [128, 32])
    bcat_d = din("bcat", [128, 32 * 16])
    bsw_d = din("bsw", [128, 32 * 16])
    ca_d = din("ca", [128, 32 * 16])
    cb_d = din("cb", [128, 32 * 16])
    dsk_d = din("dskip_t", [128, 4])
    w_glu_a_d = din("w_glu_a", [512, D])
    w_glu_b_d = din("w_glu_b", [512, D])
    w_out_d = din("w_out", [D, D])
    ln_d = {k: din(k, [1, D]) for k in ("ln1_g", "ln1_b", "ln2_g", "ln2_b", "ln3_g", "ln3_b")}
    wq_d = din("wq_c", [D, D])
    wk_d = din("wk_c", [D, D])
    wv_d = din("wv_c", [D, D])
    wo_d = din("wo_c", [D, D])
    wr_d = din("w_router", [D, NE])
    br_d = din("b_router", [1, NE])
    wgu_d = din("w_gate_up", [NE, D, 2 * D])
    bg_d = din("bg_t", [128, NE * 8])
    bl_d = din("bl_t", [128, NE * 8])
    wd_d = din("w_down", [NE, D, D])
    bd_d = din("b_down", [NE, D])
    out_d = nc.dram_tensor("out", [TOK, D], F32, kind="ExternalOutput").ap()
    cnt_d = nc.dram_tensor("cnt_out", [128, NE], F32, kind="ExternalOutput").ap()

    QK_d = dscr("s_qk", [TOK, 1152])
    V_d = dscr("s_v", [TOK, 128])
    U_d = dscr("s_u", [TOK, 512])
    G_d = nc.dram_tensor("s_g", [TOK, 2048], BF16, kind="Internal").ap()
    O_d = dscr("s_o", [TOK, D])
    ZT_d = nc.dram_tensor("s_zt", [128, 4, TOK], BF16, kind="Internal").ap()
    YDBG_d = dscr("s_ydbg", [128, 4, TOK]) if debug else None
    X2_d = dscr("s_x2", [TOK, D])
    XG_d = dscr("s_xg", [NSLOT + 128, D])
    YG_d = dscr("s_yg", [NSLOT + 128, D])

    try:
      with ExitStack() as top:
        P = Prog(nc, top)

        cur_tt = [0]

        def chk(i):
            if dbg3 >= 10 and dbg3 - 10 == i and cur_tt[0] == dbgt:
                P.flush()
                raise _Stop()

        def alloc(st, name, shape, dt):
            return TB(st.enter_context(nc.sbuf_tensor("sb_" + name, list(shape), dt)), P.buf(name))

        banks = [TB(top.enter_context(nc.psum_tensor("bank%d" % i, [128, 512], F32)), P.buf("bank%d" % i))
                 for i in range(8)]
        bank_sets = {"all": list(range(8)), "h": [0, 1, 2, 3, 4, 5], "t": [6, 7]}
        bank_ctr = {"all": 0, "h": 0, "t": 0}
        cur_set = ["all"]

        strict = [False]
        live = [False] * 8

        def bank():
            k_ = cur_set[0]
            lst = bank_sets[k_]
            idx = lst[bank_ctr[k_] % len(lst)]
            bank_ctr[k_] += 1
            if strict[0]:
                tries = 0
                while live[idx]:
                    idx = lst[bank_ctr[k_] % len(lst)]
                    bank_ctr[k_] += 1
                    tries += 1
                    assert tries <= len(lst), "all PSUM banks hold results whose consumers are not yet emitted"
                live[idx] = True
            return banks[idx]

        def rel(*bks):
            for b_ in bks:
                live[banks.index(b_)] = False

        ev_rr = [0]

        def evac(out_ap, in_ap, reads, writes, scale=None, engs=(DVE, ACT)):
            e = engs[ev_rr[0] % len(engs)]
            ev_rr[0] += 1
            if e == ACT:
                P.op(ACT, lambda en: en.activation(out=out_ap, in_=in_ap, func=AF.Copy, scale=(1.0 if scale is None else scale)), reads, writes)
            else:
                engn = e
                if scale is None:
                    P.op(engn, lambda en: en.tensor_copy(out=out_ap, in_=in_ap), reads, writes)
                else:
                    P.op(engn, lambda en: en.tensor_scalar_mul(out=out_ap, in0=in_ap, scalar1=scale), reads, writes)

        ident = alloc(top, "ident", [128, 128], F32)
        P.op(POOL, lambda e: e.memset(ident.t[:], 1.0), writes=[ident])
        P.op(POOL, lambda e: e.affine_select(out=ident.t[:], in_=ident.t[:], pattern=[[-1, 128]],
                                             compare_op=ALU.is_equal, fill=0.0, base=0, channel_multiplier=1),
             reads=[ident], writes=[ident])
        nb7 = alloc(top, "nb7", [128, 1], F32)
        P.op(POOL, lambda e: e.memset(nb7.t[:], 1.702 * 7.0), writes=[nb7])
        GK = alloc(top, "GK", [128, NT, 4], F32)
        DK = alloc(top, "DK", [128, NT, 4], I32)
        P.flush()

        def transpose_tile(dst, dst_ap_fn, src, src_ap_fn, nblk, extra_reads=()):
            b0 = 0
            while b0 < nblk:
                nb = min(4, nblk - b0)
                bk = bank()
                for j in range(nb):
                    P.op(PE, lambda e, bk=bk, j=j, b=b0 + j: e.transpose(
                        out=bk.t[:, j * 128:(j + 1) * 128], in_=src_ap_fn(b), identity=ident.t[:]),
                        reads=[src, ident] + list(extra_reads), writes=[bk])
                evac(dst_ap_fn(b0, nb), bk.t[:, 0:nb * 128].rearrange("p (k n) -> p k n", k=nb), [bk], [dst])
                rel(bk)
                b0 += nb

        def load_weight_bf16(st, name, w_ap, K, N, stage):
            nk = K // 128
            W = alloc(st, name, [128, nk, N], BF16)
            for k in range(nk):
                sg = stage[k % len(stage)]
                P.dma(SP, sg.t[:, 0:N], w_ap[k * 128:(k + 1) * 128, :], writes=[sg])
                evac(W.t[:, k, :], sg.t[:, 0:N], [sg], [W], engs=(DVE, ACT, POOL))
            return W

        def fill_weight(W, w_ap, K, N, stage):
            for k in range(K // 128):
                sg = stage[k % len(stage)]
                P.dma(SP, sg.t[:, 0:N], w_ap[k * 128:(k + 1) * 128, :], writes=[sg])
                evac(W.t[:, k, :], sg.t[:, 0:N], [sg], [W], engs=(DVE, ACT))

        def layer_norm(st_tiles, xin, g_bc, b_bc, out_tb):
            stats, mv, rstd = st_tiles
            for c in range(2):
                P.op(DVE, lambda e, c=c: e.bn_stats(out=stats.t[:, c, :], in_=xin.t[:, c * 512:(c + 1) * 512]),
                     reads=[xin], writes=[stats])
            P.op(DVE, lambda e: e.bn_aggr(out=mv.t[:], in_=stats.t[:]), reads=[stats], writes=[mv])
            P.op(DVE, lambda e: e.tensor_scalar_add(out=rstd.t[:], in0=mv.t[:, 1:2], scalar1=LN_EPS), reads=[mv], writes=[rstd])
            P.op(ACT, lambda e: e.activation(out=rstd.t[:], in_=rstd.t[:], func=AF.Ln), reads=[rstd], writes=[rstd])
            P.op(ACT, lambda e: e.activation(out=rstd.t[:], in_=rstd.t[:], func=AF.Exp, scale=-0.5), reads=[rstd], writes=[rstd])
            P.op(DVE, lambda e: e.tensor_scalar(out=out_tb.t[:], in0=xin.t[:], scalar1=mv.t[:, 0:1],
                                                scalar2=rstd.t[:, 0:1], op0=ALU.subtract, op1=ALU.mult),
                 reads=[xin, mv, rstd], writes=[out_tb])
            P.op(DVE, lambda e: e.tensor_tensor(out=out_tb.t[:], in0=out_tb.t[:], in1=g_bc.t[:], op=ALU.mult),
                 reads=[out_tb, g_bc], writes=[out_tb])
            P.op(DVE, lambda e: e.tensor_tensor(out=out_tb.t[:], in0=out_tb.t[:], in1=b_bc.t[:], op=ALU.add),
                 reads=[out_tb, b_bc], writes=[out_tb])

        sr_n = [0]

        def sin_reduced(st_tmp, out_ap, ang_ap, shift, reads, writes, tmp):
            shp = list(ang_ap.shape)
            sr_n[0] += 1
            ki = alloc(st_tmp, "sr_ki%d" % sr_n[0], shp, I32)
            kf = alloc(st_tmp, "sr_kf%d" % sr_n[0], shp, F32)
            xs = alloc(st_tmp, "sr_xs%d" % sr_n[0], shp, F32)
            P.op(DVE, lambda e: e.tensor_scalar_add(out=xs.t[:], in0=ang_ap, scalar1=shift + 2 * PI), reads=reads, writes=[xs])
            P.op(DVE, lambda e: e.tensor_scalar_mul(out=kf.t[:], in0=xs.t[:], scalar1=1.0 / (2 * PI)), reads=[xs], writes=[kf])
            P.op(DVE, lambda e: e.tensor_copy(out=ki.t[:], in_=kf.t[:]), reads=[kf], writes=[ki])
            P.op(DVE, lambda e: e.tensor_copy(out=kf.t[:], in_=ki.t[:]), reads=[ki], writes=[kf])
            P.op(DVE, lambda e: e.scalar_tensor_tensor(out=xs.t[:], in0=kf.t[:], scalar=-2 * PI, in1=xs.t[:], op0=ALU.mult, op1=ALU.add),
                 reads=[kf, xs], writes=[xs])
            P.op(DVE, lambda e: e.tensor_scalar(out=kf.t[:], in0=xs.t[:], scalar1=PI, scalar2=-2 * PI, op0=ALU.is_gt, op1=ALU.mult),
                 reads=[xs], writes=[kf])
            P.op(DVE, lambda e: e.tensor_tensor(out=xs.t[:], in0=xs.t[:], in1=kf.t[:], op=ALU.add), reads=[xs, kf], writes=[xs])
            P.op(DVE, lambda e: e.tensor_scalar(out=kf.t[:], in0=xs.t[:], scalar1=-PI, scalar2=2 * PI, op0=ALU.is_lt, op1=ALU.mult),
                 reads=[xs], writes=[kf])
            P.op(DVE, lambda e: e.tensor_tensor(out=xs.t[:], in0=xs.t[:], in1=kf.t[:], op=ALU.add), reads=[xs, kf], writes=[xs])
            P.op(DVE, lambda e: e.tensor_scalar(out=xs.t[:], in0=xs.t[:], scalar1=-PI + 1e-5, scalar2=PI - 1e-5, op0=ALU.max, op1=ALU.min),
                 reads=[xs], writes=[xs])
            P.op(ACT, lambda e: e.activation(out=out_ap, in_=xs.t[:], func=AF.Sin), reads=[xs], writes=writes)

        with ExitStack() as st:
          if 1 not in skip:
            cosr = alloc(st, "cosr", [128, NT, 32], F32)
            sinr = alloc(st, "sinr", [128, NT, 32], F32)
            Win = alloc(st, "Win", [128, 8, INW], BF16)
            with ExitStack() as st1:
                stage = [alloc(st1, "stg0", [128, INW], F32)]
                posi = alloc(st1, "posi", [128, NT], I32)
                posf = alloc(st1, "posf", [128, NT], F32)
                invf = alloc(st1, "invf", [128, 32], F32)
                iot = alloc(st1, "iot", [128, 32], I32)
                ang = alloc(st1, "ang", [128, NT, 32], F32)
                P.dma(SP, posi.t[:], pos_d[:, :], writes=[posi])
                P.op(DVE, lambda e: e.tensor_copy(out=posf.t[:], in_=posi.t[:]), reads=[posi], writes=[posf])
                P.op(POOL, lambda e: e.iota(iot.t[:], pattern=[[1, 32]], base=0, channel_multiplier=0), writes=[iot])
                P.op(DVE, lambda e: e.tensor_copy(out=invf.t[:], in_=iot.t[:]), reads=[iot], writes=[invf])
                P.op(ACT, lambda e: e.activation(out=invf.t[:], in_=invf.t[:], func=AF.Exp,
                                                 scale=-math.log(10000.0) / 32.0), reads=[invf], writes=[invf])
                P.op(DVE, lambda e: e.tensor_tensor(out=ang.t[:], in0=posf.t[:].unsqueeze(2).broadcast_to([128, NT, 32]),
                                                    in1=invf.t[:].unsqueeze(1).broadcast_to([128, NT, 32]), op=ALU.mult),
                     reads=[posf, invf], writes=[ang])
                sin_reduced(st1, cosr.t[:], ang.t[:], PI / 2, [ang], [cosr], None)
                sin_reduced(st1, sinr.t[:], ang.t[:], 0.0, [ang], [sinr], None)
                fill_weight(Win, w_in_d, D, INW, stage)
                P.flush()
            xt = [alloc(st, "xt%d" % i, [128, D], F32) for i in range(2)]
            xT = [alloc(st, "xT%d" % i, [128, 8, 128], BF16) for i in range(2)]
            pj = [alloc(st, "pj%d" % i, [128, INW], F32) for i in range(2)]
            pjb = [[TB(pj[i].t, P.buf("pj%d_%d" % (i, cb))) for cb in range(8)] for i in range(2)]
            qk = [alloc(st, "qk%d" % i, [128, 1152], F32) for i in range(2)]
            gt = [alloc(st, "gt%d" % i, [128, 2048], BF16) for i in range(2)]
            ra = alloc(st, "ra", [128, 576], F32)
            rb = alloc(st, "rb", [128, 576], F32)
            rc = alloc(st, "rc", [128, 576], F32)
            rd = alloc(st, "rd", [128, 576], F32)

            def p1_front(tt):
                s = tt % 2
                X, XT, PJ = xt[s], xT[s], pj[s]
                r0 = tt * 128
                P.dma(SP, X.t[:], x_d[r0:r0 + 128, :], writes=[X])
                transpose_tile(XT, lambda b0, nb: XT.t[:, b0:b0 + nb, :], X, lambda b: X.t[:, b * 128:(b + 1) * 128], 8)
                for cb in range(8):
                    c0 = cb * 512
                    cw = min(512, INW - c0)
                    bk = bank()
                    for k in range(8):
                        P.op(PE, lambda e: e.matmul(bk.t[:, 0:cw], lhsT=XT.t[:, k, :], rhs=Win.t[:, k, c0:c0 + cw],
                                                    start=(k == 0), stop=(k == 7)), reads=[XT, Win], writes=[bk])
                    evac(PJ.t[:, c0:c0 + cw], bk.t[:, 0:cw], [bk], [pjb[s][cb]])

            def p1_back(tt):
                s = tt % 2
                PJ, QK, GT, B_ = pj[s], qk[s], gt[s], pjb[s]
                r0 = tt * 128
                pv = PJ.t[:, 0:1152].rearrange("p (h t f) -> p h t f", h=18, t=2)
                qv = QK.t[:].rearrange("p (h t f) -> p h t f", h=18, t=2)
                cb_ = cosr.t[:, tt, :].unsqueeze(1).broadcast_to([128, 18, 32])
                sb_ = sinr.t[:, tt, :].unsqueeze(1).broadcast_to([128, 18, 32])
                r3 = lambda T_: T_.t[:].rearrange("p (h f) -> p h f", h=18)
                rq = [B_[0], B_[1], B_[2]]
                P.op(DVE, lambda e: e.tensor_tensor(out=r3(ra), in0=pv[:, :, 0, :], in1=cb_, op=ALU.mult), reads=rq + [cosr], writes=[ra])
                P.op(POOL, lambda e: e.tensor_tensor(out=r3(rb), in0=pv[:, :, 1, :], in1=sb_, op=ALU.mult), reads=rq + [sinr], writes=[rb])
                P.op(DVE, lambda e: e.tensor_tensor(out=qv[:, :, 0, :], in0=r3(ra), in1=r3(rb), op=ALU.subtract), reads=[ra, rb], writes=[QK])
                P.op(POOL, lambda e: e.tensor_tensor(out=r3(rc), in0=pv[:, :, 1, :], in1=cb_, op=ALU.mult), reads=rq + [cosr], writes=[rc])
                P.op(DVE, lambda e: e.tensor_tensor(out=r3(rd), in0=pv[:, :, 0, :], in1=sb_, op=ALU.mult), reads=rq + [sinr], writes=[rd])
                P.op(POOL, lambda e: e.tensor_tensor(out=qv[:, :, 1, :], in0=r3(rc), in1=r3(rd), op=ALU.add), reads=[rc, rd], writes=[QK])
                P.op(ACT, lambda e: e.activation(out=GT.t[:], in_=PJ.t[:, 1792:3840], func=AF.Tanh, scale=0.5), reads=B_[3:8], writes=[GT])
                P.dma(SP, U_d[r0:r0 + 128, :], PJ.t[:, 1280:1792], reads=[B_[2], B_[3]])
                P.dma(SP, G_d[r0:r0 + 128, :], GT.t[:], reads=[GT])

            mprev = alloc(st, "mprev", [128, 128], BF16)
            mcur = alloc(st, "mcur", [128, 128], BF16)
            P.op(POOL, lambda e: e.memset(mprev.t[:], 1.0), writes=[mprev])
            P.op(POOL, lambda e: e.affine_select(out=mprev.t[:], in_=mprev.t[:], pattern=[[-1, 128]],
                                                 compare_op=ALU.is_gt, fill=0.0, base=0, channel_multiplier=1),
                 reads=[mprev], writes=[mprev])
            P.op(POOL, lambda e: e.memset(mcur.t[:], 1.0), writes=[mcur])
            P.op(POOL, lambda e: e.affine_select(out=mcur.t[:], in_=mcur.t[:], pattern=[[1, 128]],
                                                 compare_op=ALU.is_ge, fill=0.0, base=0, channel_multiplier=-1),
                 reads=[mcur], writes=[mcur])
            esink = alloc(st, "esink", [128, 16], F32)
            P.dma(SP, esink.t[:], sinks_d[0, :].partition_broadcast(128), writes=[esink])
            P.op(ACT, lambda e: e.activation(out=esink.t[:], in_=esink.t[:], func=AF.Exp), reads=[esink], writes=[esink])
            qT = [alloc(st, "a_qT%d" % i, [64, 16, 128], BF16) for i in range(2)]
            kT = [alloc(st, "a_kT%d" % i, [64, 2, 128], BF16) for i in range(3)]
            va = [alloc(st, "a_va%d" % i, [128, 2, 65], BF16) for i in range(3)]
            pb = [alloc(st, "a_pb%d" % i, [128, 2, 4, 128], BF16) for i in range(2)]
            ot = [alloc(st, "a_o%d" % i, [128, D], F32) for i in range(2)]
            den = [alloc(st, "a_den%d" % i, [128, 4], F32) for i in range(2)]
            for i in range(3):
                P.op(POOL, lambda e: e.memset(va[i].t[:], 1.0), writes=[va[i]])

            def prep2(tt):
                s = tt % 2
                r0 = tt * 128
                QKt, QT, KT, VA = qk[s], qT[s], kT[tt % 3], va[tt % 3]
                for b0 in (0, 4, 8):
                    nb = min(4, 9 - b0)
                    bk = bank()
                    for j in range(nb):
                        b = b0 + j
                        P.op(PE, lambda e: e.transpose(out=bk.t[:, j * 128:(j + 1) * 128], in_=QKt.t[:, b * 128:(b + 1) * 128], identity=ident.t[:]),
                             reads=[QKt, ident], writes=[bk])
                    if b0 < 8:
                        src = bk.t[:, 0:512].rearrange("p (k n) -> p k n", k=4)
                        dv = QT.t[:, 2 * b0:2 * b0 + 8, :].rearrange("p (k two) n -> p k two n", two=2)
                        P.op(DVE, lambda e: e.tensor_copy(out=dv[:, :, 0, :], in_=src[0:64]), reads=[bk], writes=[QT])
                        P.op(ACT, lambda e: e.activation(out=dv[:, :, 1, :], in_=src[64:128], func=AF.Copy), reads=[bk], writes=[QT])
                    else:
                        P.op(DVE, lambda e: e.tensor_copy(out=KT.t[:, 0, :], in_=bk.t[0:64, 0:128]), reads=[bk], writes=[KT])
                        P.op(ACT, lambda e: e.activation(out=KT.t[:, 1, :], in_=bk.t[64:128, 0:128], func=AF.Copy), reads=[bk], writes=[KT])
                P.op(POOL, lambda e: e.tensor_copy(out=VA.t[:, :, 0:64], in_=pj[s].t[:, 1152:1280].rearrange("p (g d) -> p g d", g=2)),
                     reads=[pjb[s][2]], writes=[VA])

            def cblocks(tt):
                n = tt % 16
                return ([(kT[(tt - 1) % 3], va[(tt - 1) % 3], mprev)] if n > 0 else []) + [(kT[tt % 3], va[tt % 3], mcur)]

            def scores(tt, g):
                gk, hh = g // 2, g % 2
                h0 = 8 * gk + 4 * hh
                QT = qT[tt % 2]
                PB = pb[(tt * 4 + g) % 2]
                for ci, (KTc, VAc, msk) in enumerate(cblocks(tt)):
                    bk = bank()
                    P.op(PE, lambda e: e.matmul(bk.t[:, :], lhsT=KTc.t[:, gk, :], rhs=QT.t[:, h0:h0 + 4, :].rearrange("p h n -> p (h n)"),
                                                start=True, stop=True), reads=[KTc, QT], writes=[bk])
                    P.op(ACT, lambda e: e.activation(out=PB.t[:, ci, :, :].rearrange("p h n -> p (h n)"), in_=bk.t[:, :], func=AF.Exp, scale=0.125),
                         reads=[bk], writes=[PB])
                    P.op(DVE, lambda e: e.tensor_tensor(out=PB.t[:, ci, :, :], in0=PB.t[:, ci, :, :],
                                                        in1=msk.t[:].unsqueeze(1).broadcast_to([128, 4, 128]), op=ALU.mult),
                         reads=[PB, msk], writes=[PB])

            def pv(tt, g):
                gk, hh = g // 2, g % 2
                h0 = 8 * gk + 4 * hh
                OT = ot[tt % 2]
                PB = pb[(tt * 4 + g) % 2]
                DEN = den[g % 2]
                cbs = cblocks(tt)
                ob = bank()
                for j in range(4):
                    for ci, (KTc, VAc, msk) in enumerate(cbs):
                        P.op(PE, lambda e: e.matmul(ob.t[:, j * 65:(j + 1) * 65], lhsT=PB.t[:, ci, j, :], rhs=VAc.t[:, gk, :],
                                                    start=(ci == 0), stop=(ci == len(cbs) - 1)), reads=[PB, VAc], writes=[ob])
                ov = ob.t[:, 0:260].rearrange("p (h d) -> p h d", h=4)
                P.op(DVE, lambda e: e.tensor_tensor(out=DEN.t[:], in0=ov[:, :, 64], in1=esink.t[:, h0:h0 + 4], op=ALU.add),
                     reads=[ob, esink], writes=[DEN])
                P.op(DVE, lambda e: e.reciprocal(out=DEN.t[:], in_=DEN.t[:]), reads=[DEN], writes=[DEN])
                P.op(DVE, lambda e: e.tensor_tensor(out=OT.t[:, h0 * 64:(h0 + 4) * 64].rearrange("p (h d) -> p h d", h=4), in0=ov[:, :, 0:64],
                                                    in1=DEN.t[:].unsqueeze(2).broadcast_to([128, 4, 64]), op=ALU.mult),
                     reads=[ob, DEN], writes=[OT])


            zfill = alloc(st, "zfill", [128, D], F32)
            P.op(POOL, lambda e: e.memset(zfill.t[:], 0.0), writes=[zfill])
            for a_ in range(NSLOT // 1024):
                P.dma(POOL, XG_d[a_ * 1024:(a_ + 1) * 1024, :].rearrange("(a p) d -> p a d", p=128),
                      zfill.t[:].unsqueeze(1).broadcast_to([128, 8, D]), reads=[zfill])

            p1_front(0)
            for tt in range(NT):
                if tt + 1 < NT:
                    p1_front(tt + 1)
                p1_back(tt)
                prep2(tt)
                scores(tt, 0)
                for g in range(4):
                    if g < 3:
                        scores(tt, g + 1)
                    pv(tt, g)
                P.dma(SP, O_d[tt * 128:(tt + 1) * 128, :], ot[tt % 2].t[:], reads=[ot[tt % 2]])
            P.flush()

        if nphase < 2:
            return nc

        if nphase < 3:
            return nc
        with ExitStack() as st:
            A = lambda name, shape, dt=F32: alloc(st, name, shape, dt)
            LR, LI, LDT = A("LR", [128, 32]), A("LI", [128, 32]), A("LDT", [128, 32])
            for tb_, d_ in ((LR, lr_d), (LI, li_d), (LDT, ldt_d)):
                P.dma(SP, tb_.t[:], d_[:, :], writes=[tb_])
            BCAT, BSW = A("BCAT", [128, 32, 16]), A("BSW", [128, 32, 16])
            CA, CB = A("CA", [128, 32, 16]), A("CB", [128, 32, 16])
            for tb_, d_ in ((BCAT, bcat_d), (BSW, bsw_d), (CA, ca_d), (CB, cb_d)):
                P.dma(SP, tb_.t[:].rearrange("p g h -> p (g h)"), d_[:, :], writes=[tb_])
            DSK = A("DSK", [128, 4])
            P.dma(SP, DSK.t[:], dsk_d[:, :], writes=[DSK])
            sgn = A("sgn", [128, 1])
            P.op(POOL, lambda e: e.memset(sgn.t[0:64, :], 1.0), writes=[sgn])
            P.op(POOL, lambda e: e.memset(sgn.t[64:128, :], -1.0), writes=[sgn])
            names = ["dt", "mag", "th", "cs", "sn", "are", "aim", "den", "t1", "t2", "fre", "fim", "tmp", "nfre", "nfims", "fims"]
            S_ = {n_: A("s_" + n_, [128, 32]) for n_ in names}
            tt2 = lambda o, a, b, op, eng=DVE: P.op(eng, lambda e: e.tensor_tensor(out=S_[o].t[:], in0=S_[a].t[:] if isinstance(a, str) else a.t[:],
                                                                                  in1=S_[b].t[:] if isinstance(b, str) else b.t[:], op=op),
                                                    reads=[S_[a] if isinstance(a, str) else a, S_[b] if isinstance(b, str) else b], writes=[S_[o]])
            P.op(ACT, lambda e: e.activation(out=S_["dt"].t[:], in_=LDT.t[:], func=AF.Exp), reads=[LDT], writes=[S_["dt"]])
            tt2("mag", LR, "dt", ALU.mult)
            P.op(ACT, lambda e: e.activation(out=S_["mag"].t[:], in_=S_["mag"].t[:], func=AF.Exp), reads=[S_["mag"]], writes=[S_["mag"]])
            tt2("th", LI, "dt", ALU.mult)
            sin_reduced(st, S_["cs"].t[:], S_["th"].t[:], PI / 2, [S_["th"]], [S_["cs"]], (S_["tmp"].t[:], S_["tmp"]))
            sin_reduced(st, S_["sn"].t[:], S_["th"].t[:], 0.0, [S_["th"]], [S_["sn"]], (S_["tmp"].t[:], S_["tmp"]))
            tt2("are", "mag", "cs", ALU.mult)
            tt2("aim", "mag", "sn", ALU.mult)
            P.op(DVE, lambda e: e.tensor_scalar_add(out=S_["are"].t[:], in0=S_["are"].t[:], scalar1=-1.0), reads=[S_["are"]], writes=[S_["are"]])
            tt2("den", LR, LR, ALU.mult)
            tt2("t1", LI, LI, ALU.mult)
            tt2("den", "den", "t1", ALU.add)
            P.op(DVE, lambda e: e.reciprocal(out=S_["den"].t[:], in_=S_["den"].t[:]), reads=[S_["den"]], writes=[S_["den"]])
            tt2("t1", "are", LR, ALU.mult)
            tt2("t2", "aim", LI, ALU.mult)
            tt2("fre", "t1", "t2", ALU.add)
            tt2("fre", "fre", "den", ALU.mult)
            tt2("t1", "aim", LR, ALU.mult)
            tt2("t2", "are", LI, ALU.mult)
            tt2("fim", "t1", "t2", ALU.subtract)
            tt2("fim", "fim", "den", ALU.mult)
            P.op(DVE, lambda e: e.tensor_scalar(out=S_["nfre"].t[:], in0=S_["fre"].t[:], scalar1=sgn.t[:, 0:1], scalar2=None, op0=ALU.mult),
                 reads=[S_["fre"], sgn], writes=[S_["nfre"]])
            P.op(DVE, lambda e: e.tensor_scalar(out=S_["fims"].t[:], in0=S_["fim"].t[:], scalar1=sgn.t[:, 0:1], scalar2=-1.0, op0=ALU.mult, op1=ALU.mult),
                 reads=[S_["fim"], sgn], writes=[S_["fims"]])
            B1, B2, BT = A("B1", [128, 32, 16]), A("B2", [128, 32, 16]), A("BT", [128, 32, 16])
            bc = lambda n_: S_[n_].t[:].unsqueeze(2).broadcast_to([128, 32, 16])
            P.op(DVE, lambda e: e.tensor_tensor(out=B1.t[:], in0=BCAT.t[:], in1=bc("fre"), op=ALU.mult), reads=[BCAT, S_["fre"]], writes=[B1])
            P.op(DVE, lambda e: e.tensor_tensor(out=BT.t[:], in0=BSW.t[:], in1=bc("fims"), op=ALU.mult), reads=[BSW, S_["fims"]], writes=[BT])
            P.op(DVE, lambda e: e.tensor_tensor(out=B1.t[:], in0=B1.t[:], in1=BT.t[:], op=ALU.add), reads=[B1, BT], writes=[B1])
            P.op(DVE, lambda e: e.tensor_tensor(out=B2.t[:], in0=BSW.t[:], in1=bc("nfre"), op=ALU.mult), reads=[BSW, S_["nfre"]], writes=[B2])
            P.op(DVE, lambda e: e.tensor_tensor(out=BT.t[:], in0=BCAT.t[:], in1=bc("fim"), op=ALU.mult), reads=[BCAT, S_["fim"]], writes=[BT])
            P.op(DVE, lambda e: e.tensor_tensor(out=B2.t[:], in0=B2.t[:], in1=BT.t[:], op=ALU.add), reads=[B2, BT], writes=[B2])
            rmask = A("rmask", [128, 8])
            P.op(POOL, lambda e: e.memset(rmask.t[:], 1.0), writes=[rmask])
            P.op(POOL, lambda e: e.affine_select(out=rmask.t[:], in_=rmask.t[:], pattern=[[-16, 8]], compare_op=ALU.is_ge,
                                                 fill=0.0, base=0, channel_multiplier=1), reads=[rmask], writes=[rmask])
            P.op(POOL, lambda e: e.affine_select(out=rmask.t[:], in_=rmask.t[:], pattern=[[16, 8]], compare_op=ALU.is_ge,
                                                 fill=0.0, base=15, channel_multiplier=-1), reads=[rmask], writes=[rmask])
            B1z, B2z = A("B1z", [128, 32, 128], BF16), A("B2z", [128, 32, 128], BF16)
            C1z, C2z = A("C1z", [128, 32, 128], BF16), A("C2z", [128, 32, 128], BF16)
            for Bsrc, Bz in ((B1, B1z), (B2, B2z)):
                for g4 in range(4):
                    bk = bank()
                    P.op(PE, lambda e, bk=bk, Bsrc=Bsrc, g4=g4: e.transpose(
                        out=bk.t[:, 0:128], in_=Bsrc.t[:, g4 * 8:(g4 + 1) * 8, :].rearrange("p g h -> p (g h)"), identity=ident.t[:]),
                        reads=[Bsrc, ident], writes=[bk])
                    for g8 in range(8):
                        P.op(DVE, lambda e, bk=bk, Bz=Bz, g=g4 * 8 + g8, g8=g8: e.tensor_scalar(
                            out=Bz.t[:, g, :], in0=bk.t[:, 0:128], scalar1=rmask.t[:, g8:g8 + 1], scalar2=None, op0=ALU.mult),
                            reads=[bk, rmask], writes=[Bz])
            P.op(POOL, lambda e: e.memset(C1z.t[:], 0.0), writes=[C1z])
            P.op(POOL, lambda e: e.memset(C2z.t[:], 0.0), writes=[C2z])
            for g in range(32):
                g8 = g % 8
                P.op(DVE, lambda e, g=g, g8=g8: e.tensor_scalar(out=C1z.t[:, g, g8 * 16:(g8 + 1) * 16], in0=CA.t[:, g, :],
                                                                scalar1=sgn.t[:, 0:1], scalar2=None, op0=ALU.mult),
                     reads=[CA, sgn], writes=[C1z])
                P.op(POOL, lambda e, g=g, g8=g8: e.tensor_scalar(out=C2z.t[:, g, g8 * 16:(g8 + 1) * 16], in0=CB.t[:, g, :],
                                                                 scalar1=-1.0, scalar2=None, op0=ALU.mult),
                     reads=[CB], writes=[C2z])
            jj_i = A("jj_i", [128, 128], I32)
            jj = A("jj", [128, 128])
            P.op(POOL, lambda e: e.iota(jj_i.t[:], pattern=[[1, 128]], base=0, channel_multiplier=0), writes=[jj_i])
            P.op(DVE, lambda e: e.tensor_copy(out=jj.t[:], in_=jj_i.t[:]), reads=[jj_i], writes=[jj])
            cosS, sinS, rfull = A("cosS", [128, 32, 128]), A("sinS", [128, 32, 128]), A("rfull", [128, 32, 128])
            with ExitStack() as stt:
                angS = alloc(stt, "angS", [128, 32, 128], F32)
                for g in range(32):
                    P.op(DVE, lambda e, g=g: e.tensor_scalar(out=angS.t[:, g, :], in0=jj.t[:], scalar1=S_["th"].t[:, g:g + 1],
                                                            scalar2=None, op0=ALU.mult), reads=[jj, S_["th"]], writes=[angS])
                    P.op(POOL, lambda e, g=g: e.tensor_copy(out=rfull.t[:, g, :], in_=S_["mag"].t[:, g:g + 1].broadcast_to([128, 128])),
                         reads=[S_["mag"]], writes=[rfull])
                with ExitStack() as stt2:
                    sin_reduced(stt2, cosS.t[:], angS.t[:], PI / 2, [angS], [cosS], None)
                    P.flush()
                with ExitStack() as stt2:
                    sin_reduced(stt2, sinS.t[:], angS.t[:], 0.0, [angS], [sinS], None)
                    P.flush()
            P.op(POOL, lambda e: e.memset(rfull.t[:, :, 0], 0.0), reads=[rfull], writes=[rfull])
            rc = A("rcar", [128, 32])
            thL = A("thL", [128, 32])
            cosL, sinLs = A("cosL", [128, 32]), A("sinLs", [128, 32])
            P.op(DVE, lambda e: e.tensor_scalar_mul(out=thL.t[:], in0=S_["th"].t[:], scalar1=128.0), reads=[S_["th"]], writes=[thL])
            sin_reduced(st, cosL.t[:], thL.t[:], PI / 2, [thL], [cosL], (S_["tmp"].t[:], S_["tmp"]))
            sin_reduced(st, sinLs.t[:], thL.t[:], 0.0, [thL], [sinLs], (S_["tmp"].t[:], S_["tmp"]))
            P.op(DVE, lambda e: e.tensor_scalar(out=sinLs.t[:], in0=sinLs.t[:], scalar1=sgn.t[:, 0:1], scalar2=-1.0, op0=ALU.mult, op1=ALU.mult),
                 reads=[sinLs, sgn], writes=[sinLs])
            permS = A("permS", [128, 128])
            P.op(POOL, lambda e: e.tensor_copy(out=permS.t[:, 0:64], in_=ident.t[:, 64:128]), reads=[ident], writes=[permS])
            P.op(POOL, lambda e: e.tensor_copy(out=permS.t[:, 64:128], in_=ident.t[:, 0:64]), reads=[ident], writes=[permS])

            if dbg3 == 1:
                P.flush()
                return nc
            ut = [A("u_t%d" % i, [128, 512]) for i in range(2)]
            uTb = [A("uTb%d" % i, [128, 4, 128], BF16) for i in range(2)]
            uTf = [A("uTf%d" % i, [128, 4, 128]) for i in range(2)]
            rot = [A("rot%d" % i, [128, 8, 128]) for i in range(2)]
            t1s = A("t1s", [128, 512])
            t2s = A("t2s", [128, 512])
            Gs = [A("Gs%d" % i, [128, 8, 128]) for i in range(2)]
            hc = [A("hc%d" % i, [128, 8, 128], BF16) for i in range(2)]
            hs = [A("hs%d" % i, [128, 8, 128], BF16) for i in range(2)]
            yv = [A("yv%d" % i, [128, 4, 128]) for i in range(2)]
            ge1, ge2 = A("ge1", [128, 512]), A("ge2", [128, 512])
            zt = [A("zt%d" % i, [128, 4, 128], BF16) for i in range(2)]
            last = A("last", [128, 32])
            swp = A("swp", [128, 32])
            carry = A("carry", [128, 32])
            def prep(tt):
                s = tt % 2
                UT, UB, UF = ut[s], uTb[s], uTf[s]
                P.dma(SP, UT.t[:], U_d[tt * 128:(tt + 1) * 128, :], writes=[UT])
                bk = bank()
                for j in range(4):
                    P.op(PE, lambda e: e.transpose(out=bk.t[:, j * 128:(j + 1) * 128], in_=UT.t[:, j * 128:(j + 1) * 128],
                                                   identity=ident.t[:]), reads=[UT, ident], writes=[bk])
                P.op(DVE, lambda e: e.tensor_copy(out=UB.t[:].rearrange("p k n -> p (k n)"), in_=bk.t[:, :]), reads=[bk], writes=[UB])
                P.op(DVE, lambda e: e.tensor_copy(out=UF.t[:].rearrange("p k n -> p (k n)"), in_=bk.t[:, :]), reads=[bk], writes=[UF])

            prep(0)
            for tt in range(NT):
                s = tt % 2
                n = tt % 16
                r0 = tt * 128
                UT, UB, UF, YV, ZT = ut[s], uTb[s], uTf[s], yv[s], zt[s]
                if n == 0:
                    P.op(POOL, lambda e: e.memset(carry.t[:], 0.0), writes=[carry])
                P.op(DVE, lambda e: e.tensor_tensor(out=rc.t[:], in0=carry.t[:], in1=S_["mag"].t[:], op=ALU.mult), reads=[carry, S_["mag"]], writes=[rc])

                def stageA(Q):
                    ROT = rot[Q % 2]
                    for half in range(2):
                        b1, b2 = bank(), bank()
                        for j in range(4):
                            g = Q * 8 + half * 4 + j
                            P.op(PE, lambda e: e.matmul(b1.t[:, j * 128:(j + 1) * 128], lhsT=B1z.t[:, g, :], rhs=UB.t[:, Q, :], start=True, stop=True),
                                 reads=[B1z, UB], writes=[b1])
                        for j in range(4):
                            g = Q * 8 + half * 4 + j
                            P.op(PE, lambda e: e.matmul(b2.t[:, j * 128:(j + 1) * 128], lhsT=B2z.t[:, g, :], rhs=UB.t[:, Q, :], start=True, stop=True),
                                 reads=[B2z, UB], writes=[b2])
                        g0 = Q * 8 + half * 4
                        P.op(DVE, lambda e: e.tensor_tensor(out=t1s.t[:], in0=b1.t[:, :], in1=cosS.t[:, g0:g0 + 4, :].rearrange("p g n -> p (g n)"), op=ALU.mult),
                             reads=[b1, cosS], writes=[t1s])
                        P.op(DVE, lambda e: e.tensor_tensor(out=t2s.t[:], in0=b2.t[:, :], in1=sinS.t[:, g0:g0 + 4, :].rearrange("p g n -> p (g n)"), op=ALU.mult),
                             reads=[b2, sinS], writes=[t2s])
                        P.op(DVE, lambda e: e.tensor_tensor(out=ROT.t[:, half * 4:(half + 1) * 4, :].rearrange("p g n -> p (g n)"), in0=t1s.t[:], in1=t2s.t[:], op=ALU.add),
                             reads=[t1s, t2s], writes=[ROT])

                def stageB(Q):
                    ROT, GS, HC, HS = rot[Q % 2], Gs[Q % 2], hc[Q % 2], hs[Q % 2]
                    P.op(DVE, lambda e: e.tensor_tensor(out=ROT.t[:, :, 0], in0=ROT.t[:, :, 0], in1=rc.t[:, Q * 8:(Q + 1) * 8], op=ALU.add),
                         reads=[ROT, rc], writes=[ROT])
                    P.op(DVE, lambda e: e.tensor_tensor_scan(out=GS.t[:].rearrange("p g n -> p (g n)"),
                                                             data0=rfull.t[:, Q * 8:(Q + 1) * 8, :].rearrange("p g n -> p (g n)"),
                                                             data1=ROT.t[:].rearrange("p g n -> p (g n)"), initial=0.0,
                                                             op0=ALU.mult, op1=ALU.add), reads=[rfull, ROT], writes=[GS])
                    P.op(POOL, lambda e: e.tensor_tensor(out=HS.t[:], in0=GS.t[:], in1=sinS.t[:, Q * 8:(Q + 1) * 8, :], op=ALU.mult),
                         reads=[GS, sinS], writes=[HS])
                    P.op(DVE, lambda e: e.tensor_tensor(out=HC.t[:], in0=GS.t[:], in1=cosS.t[:, Q * 8:(Q + 1) * 8, :], op=ALU.mult),
                         reads=[GS, cosS], writes=[HC])
                    P.op(ACT, lambda e: e.activation(out=last.t[:, Q * 8:(Q + 1) * 8], in_=GS.t[:, :, 127], func=AF.Copy), reads=[GS], writes=[last])

                def stageC(Q):
                    HC, HS = hc[Q % 2], hs[Q % 2]
                    yb = bank()
                    for j in range(8):
                        g = Q * 8 + j
                        P.op(PE, lambda e: e.matmul(yb.t[:, 0:128], lhsT=C1z.t[:, g, :], rhs=HC.t[:, j, :], start=(j == 0), stop=False),
                             reads=[C1z, HC], writes=[yb])
                        P.op(PE, lambda e: e.matmul(yb.t[:, 0:128], lhsT=C2z.t[:, g, :], rhs=HS.t[:, j, :], start=False, stop=(j == 7)),
                             reads=[C2z, HS], writes=[yb])
                    P.op(DVE, lambda e: e.scalar_tensor_tensor(out=YV.t[:, Q, :], in0=UF.t[:, Q, :], scalar=DSK.t[:, Q:Q + 1], in1=yb.t[:, 0:128],
                                                               op0=ALU.mult, op1=ALU.add), reads=[yb, UF, DSK], writes=[YV])

                stageA(0)
                stageA(1)
                if tt + 1 < NT:
                    prep(tt + 1)
                stageB(0)
                stageA(2)
                stageB(1)
                stageC(0)
                stageA(3)
                stageB(2)
                stageC(1)
                stageB(3)
                stageC(2)
                stageC(3)
                P.op(DVE, lambda e: e.tensor_copy(out=swp.t[0:64, :], in_=last.t[64:128, :]), reads=[last], writes=[swp])
                P.op(DVE, lambda e: e.tensor_copy(out=swp.t[64:128, :], in_=last.t[0:64, :]), reads=[last], writes=[swp])
                P.op(DVE, lambda e: e.tensor_tensor(out=swp.t[:], in0=swp.t[:], in1=sinLs.t[:], op=ALU.mult),
                     reads=[swp, sinLs], writes=[swp])
                P.op(DVE, lambda e: e.tensor_tensor(out=carry.t[:], in0=last.t[:], in1=cosL.t[:], op=ALU.mult),
                     reads=[last, cosL], writes=[carry])
                P.op(DVE, lambda e: e.tensor_tensor(out=carry.t[:], in0=carry.t[:], in1=swp.t[:], op=ALU.add),
                     reads=[carry, swp], writes=[carry])
                chk(5)
                yf = YV.t[:].rearrange("p k n -> p (k n)")
                P.op(DVE, lambda e, yf=yf: e.tensor_tensor(out=ge1.t[:], in0=yf, in1=yf, op=ALU.mult), reads=[YV], writes=[ge1])
                P.op(DVE, lambda e: e.tensor_scalar(out=ge1.t[:], in0=ge1.t[:], scalar1=0.044715, scalar2=1.0, op0=ALU.mult, op1=ALU.add),
                     reads=[ge1], writes=[ge1])
                P.op(DVE, lambda e, yf=yf: e.tensor_tensor(out=ge1.t[:], in0=ge1.t[:], in1=yf, op=ALU.mult), reads=[ge1, YV], writes=[ge1])
                P.op(ACT, lambda e: e.activation(out=ge2.t[:], in_=ge1.t[:], func=AF.Tanh, scale=math.sqrt(2.0 / PI)), reads=[ge1], writes=[ge2])
                P.op(DVE, lambda e: e.tensor_scalar(out=ge2.t[:], in0=ge2.t[:], scalar1=0.5, scalar2=0.5, op0=ALU.mult, op1=ALU.add),
                     reads=[ge2], writes=[ge2])
                P.op(DVE, lambda e, yf=yf, ZT=ZT: e.tensor_tensor(out=ZT.t[:].rearrange("p k n -> p (k n)"), in0=ge2.t[:], in1=yf, op=ALU.mult),
                     reads=[ge2, YV], writes=[ZT])
                chk(6)
                P.dma(SP, ZT_d[:, :, r0:r0 + 128], ZT.t[:], reads=[ZT])
                chk(7)
                if debug:
                    P.dma(SP, YDBG_d[:, :, r0:r0 + 128], YV.t[:], reads=[YV])
                chk(8)
                if dbg3 == 3:
                    P.flush()
            P.flush()

        if nphase < 4:
            return nc
        with ExitStack() as st:
            A = lambda name, shape, dt=F32: alloc(st, name, shape, dt)
            lnp = {}
            for k_ in ("ln1_g", "ln1_b", "ln2_g", "ln2_b"):
                lnp[k_] = A(k_, [128, D])
                P.dma(SP, lnp[k_].t[:], ln_d[k_][0, :].partition_broadcast(128), writes=[lnp[k_]])
            brb = A("brb", [128, NE])
            P.dma(SP, brb.t[:], br_d[0, :].partition_broadcast(128), writes=[brb])
            wr = A("wr", [128, 8, NE])
            P.dma(SP, wr.t[:], wr_d.rearrange("(k p) n -> p k n", p=128), writes=[wr])
            triu = A("triu", [128, 128], BF16)
            P.op(POOL, lambda e: e.memset(triu.t[:], 1.0), writes=[triu])
            P.op(POOL, lambda e: e.affine_select(out=triu.t[:], in_=triu.t[:], pattern=[[1, 128]], compare_op=ALU.is_gt,
                                                 fill=0.0, base=0, channel_multiplier=-1), reads=[triu], writes=[triu])
            ones_b = A("ones_b", [128, 128], BF16)
            P.op(POOL, lambda e: e.memset(ones_b.t[:], 1.0), writes=[ones_b])
            ecap = A("ecap", [128, NE])
            ecap_i = A("ecap_i", [128, NE], I32)
            P.op(POOL, lambda e: e.iota(ecap_i.t[:], pattern=[[CAP, NE]], base=0, channel_multiplier=0), writes=[ecap_i])
            P.op(DVE, lambda e: e.tensor_copy(out=ecap.t[:], in_=ecap_i.t[:]), reads=[ecap_i], writes=[ecap])
            junk_i = A("junk_i", [128, 1], I32)
            junk = A("junk", [128, 1])
            P.op(POOL, lambda e: e.iota(junk_i.t[:], pattern=[[0, 1]], base=NSLOT, channel_multiplier=1), writes=[junk_i])
            P.op(DVE, lambda e: e.tensor_copy(out=junk.t[:], in_=junk_i.t[:]), reads=[junk_i], writes=[junk])
            cnt_run = A("cnt_run", [128, NE])
            P.op(POOL, lambda e: e.memset(cnt_run.t[:], 0.0), writes=[cnt_run])
            KcT = [A("KcT%d" % i, [128, 8, 256], BF16) for i in range(NSEQ)]
            Vc = [A("Vc%d" % i, [128, 2, D], BF16) for i in range(NSEQ)]
            with ExitStack() as st2:
                stage = [alloc(st2, "stg4_%d" % i, [128, 1024], F32) for i in range(2)]
                P.op(POOL, lambda e: e.memset(stage[0].t[:], 0.0), writes=[stage[0]])
                P.dma(SP, YG_d[NSLOT:NSLOT + 128, :], stage[0].t[:], reads=[stage[0]])
                Wk = load_weight_bf16(st2, "Wk", wk_d, D, D, stage)
                Wv = load_weight_bf16(st2, "Wv", wv_d, D, D, stage)
                memt = [alloc(st2, "memt%d" % i, [128, D], F32) for i in range(2)]
                memT = alloc(st2, "memT", [128, 8, 256], BF16)
                for sq in range(NSEQ):
                    for mt in range(2):
                        M_ = memt[mt]
                        P.dma(SP, M_.t[:], mem_d[sq * 256 + mt * 128: sq * 256 + (mt + 1) * 128, :], writes=[M_])
                        transpose_tile(memT, lambda b0, nb, mt=mt: memT.t[:, b0:b0 + nb, mt * 128:(mt + 1) * 128], M_,
                                       lambda b, M_=M_: M_.t[:, b * 128:(b + 1) * 128], 8)
                    for j in range(8):
                        bk = bank()
                        for k in range(8):
                            P.op(PE, lambda e, bk=bk, k=k, j=j: e.matmul(bk.t[:, 0:256], lhsT=Wk.t[:, k, j * 128:(j + 1) * 128],
                                                                         rhs=memT.t[:, k, :], start=(k == 0), stop=(k == 7)),
                                 reads=[Wk, memT], writes=[bk])
                        evac(KcT[sq].t[:, j, :], bk.t[:, 0:256], [bk], [KcT[sq]])
                    for mt in range(2):
                        for hf in range(2):
                            bk = bank()
                            for k in range(8):
                                P.op(PE, lambda e, bk=bk, k=k, mt=mt, hf=hf: e.matmul(
                                    bk.t[:, :], lhsT=memT.t[:, k, mt * 128:(mt + 1) * 128], rhs=Wv.t[:, k, hf * 512:(hf + 1) * 512],
                                    start=(k == 0), stop=(k == 7)), reads=[Wv, memT], writes=[bk])
                            evac(Vc[sq].t[:, mt, hf * 512:(hf + 1) * 512], bk.t[:, :], [bk], [Vc[sq]])
                P.flush()
            Wo, Wa, Wb = A("Wo", [128, 8, D], BF16), A("Wa", [128, 4, D], BF16), A("Wb", [128, 4, D], BF16)
            Wout, Wq, Woc = A("Wout", [128, 8, D], BF16), A("Wq", [128, 8, D], BF16), A("Woc", [128, 8, D], BF16)
            with ExitStack() as stw:
                stage_w = [alloc(stw, "stgw%d" % i, [128, 1024], F32) for i in range(2)]
                for W_, d_, K_ in ((Wo, w_attn_o_d, D), (Wa, w_glu_a_d, 512), (Wb, w_glu_b_d, 512), (Wout, w_out_d, D), (Wq, wq_d, D), (Woc, wo_d, D)):
                    fill_weight(W_, d_, K_, D, stage_w)
                P.flush()
            lanes = []
            for L in range(3):
                n_ = lambda x: "p4_%s%d" % (x, L)
                lanes.append(dict(
                    Z=A(n_("z"), [128, 4, 128], BF16), G=A(n_("g"), [128, 2048], BF16),
                    aT=A(n_("aT"), [128, 8, 128], BF16), tb=A(n_("tb"), [128, D]), m1=A(n_("m1"), [128, D]), xin=A(n_("xin"), [128, D]),
                    qcT=A(n_("qcT"), [128, 8, 128], BF16), pe=A(n_("pe"), [128, 4, 256]), pT=A(n_("pT"), [128, 8, 128], BF16),
                    stats=A(n_("stats"), [128, 2, 6]), mv=A(n_("mv"), [128, 2]), rstd=A(n_("rstd"), [128, 1]),
                    mx=A(n_("mx"), [128, 4]), nmx=A(n_("nmx"), [128, 4]), ssum=A(n_("ssum"), [128, 4])))
            x2T = A("p4_x2T", [128, 8, 128])
            lg = A("p4_lg", [128, NE])
            top8 = A("p4_top8", [128, 8])
            msk = A("p4_msk", [128, NE])
            mskb = A("p4_mskb", [128, NE], BF16)
            ex = A("p4_ex", [128, NE])
            nv0 = A("p4_nv0", [128, 1])
            esum = A("p4_esum", [128, 1])
            gate = A("p4_gate", [128, NE])
            dest = A("p4_dest", [128, NE])
            oh = A("p4_oh", [128, NE])
            ohd = A("p4_ohd", [128, NE])
            dkf = A("p4_dkf", [128, 4])

            def linear2(lhsT_tb, nk, W):
                bks = [bank(), bank()]
                for hf in range(2):
                    for k in range(nk):
                        P.op(PE, lambda e: e.matmul(bks[hf].t[:, :], lhsT=lhsT_tb.t[:, k, :], rhs=W.t[:, k, hf * 512:(hf + 1) * 512],
                                                    start=(k == 0), stop=(k == nk - 1)), reads=[lhsT_tb, W], writes=[bks[hf]])
                return bks

            def make_tile(tt):
                d = lanes[tt % 3]
                sq = tt // 16
                r0 = tt * 128
                Z_, G_, aT, tb, m1, xin = d["Z"], d["G"], d["aT"], d["tb"], d["m1"], d["xin"]
                qcT, pe_, pT, X2 = d["qcT"], d["pe"], d["pT"], d["xin"]
                O_ap = pe_.t[:].rearrange("p h m -> p (h m)")
                lnt = (d["stats"], d["mv"], d["rstd"])
                mx, nmx, ssum = d["mx"], d["nmx"], d["ssum"]
                S = {}
                HS = [slice(0, 512), slice(512, 1024)]

                def to_T(src):
                    transpose_tile(aT, lambda b0, nb: aT.t[:, b0:b0 + nb, :], src, lambda b: src.t[:, b * 128:(b + 1) * 128], 8)

                def s0():
                    P.dma(SP, O_ap, O_d[r0:r0 + 128, :], writes=[pe_])
                    P.dma(SP, Z_.t[:], ZT_d[:, :, r0:r0 + 128], writes=[Z_])
                    P.dma(SP, G_.t[:], G_d[r0:r0 + 128, :], writes=[G_])
                    P.dma(SP, xin.t[:], x_d[r0:r0 + 128, :], writes=[xin])
                    transpose_tile(aT, lambda b0, nb: aT.t[:, b0:b0 + nb, :], pe_, lambda b: O_ap[:, b * 128:(b + 1) * 128], 8)

                def s1():
                    S["ba"] = linear2(aT, 8, Wo)

                def s1b():
                    ba = S["ba"]
                    for hf in range(2):
                        sl = HS[hf]
                        P.op(DVE, lambda e: e.scalar_tensor_tensor(out=m1.t[:, sl], in0=G_.t[:, sl], scalar=1.0, in1=ba[hf].t[:, :],
                                                                   op0=ALU.add, op1=ALU.mult), reads=[G_, ba[hf]], writes=[m1])
                    rel(*ba)
                    S["bgb"] = linear2(Z_, 4, Wb)

                def s2():
                    bgb = S["bgb"]
                    for hf in range(2):
                        sl = HS[hf]
                        P.op(ACT, lambda e: e.activation(out=tb.t[:, sl], in_=bgb[hf].t[:, :], func=AF.Tanh, scale=0.5), reads=[bgb[hf]], writes=[tb])
                    rel(*bgb)
                    S["bga"] = linear2(Z_, 4, Wa)

                def s3():
                    bga = S["bga"]
                    for hf in range(2):
                        sl = HS[hf]
                        P.op(DVE, lambda e: e.scalar_tensor_tensor(out=tb.t[:, sl], in0=tb.t[:, sl], scalar=1.0, in1=bga[hf].t[:, :],
                                                                   op0=ALU.add, op1=ALU.mult), reads=[tb, bga[hf]], writes=[tb])
                    rel(*bga)
                    P.op(DVE, lambda e: e.scalar_tensor_tensor(out=tb.t[:], in0=G_.t[:, 1024:2048], scalar=1.0, in1=tb.t[:],
                                                               op0=ALU.add, op1=ALU.mult), reads=[G_, tb], writes=[tb])
                    P.op(DVE, lambda e: e.scalar_tensor_tensor(out=m1.t[:], in0=tb.t[:], scalar=0.5, in1=m1.t[:],
                                                               op0=ALU.mult, op1=ALU.add), reads=[tb, m1], writes=[m1])
                    to_T(m1)

                def s4():
                    S["bm"] = linear2(aT, 8, Wout)
                    P.op(ACT, lambda e: e.activation(out=xin.t[:], in_=xin.t[:], func=AF.Copy, scale=ALPHA), reads=[xin], writes=[xin])

                def s5():
                    bm = S["bm"]
                    for hf in range(2):
                        sl = HS[hf]
                        P.op(DVE, lambda e: e.scalar_tensor_tensor(out=xin.t[:, sl], in0=bm[hf].t[:, :], scalar=0.5, in1=xin.t[:, sl],
                                                                   op0=ALU.mult, op1=ALU.add), reads=[bm[hf], xin], writes=[xin])
                    rel(*bm)
                    layer_norm(lnt, xin, lnp["ln1_g"], lnp["ln1_b"], tb)
                    to_T(tb)

                def s6():
                    for j2 in range(2):
                        bq = bank()
                        for j in range(4 * j2, 4 * j2 + 4):
                            for k in range(8):
                                P.op(PE, lambda e: e.matmul(bq.t[:, (j % 4) * 128:(j % 4 + 1) * 128], lhsT=Wq.t[:, k, j * 128:(j + 1) * 128],
                                                            rhs=aT.t[:, k, :], start=(k == 0), stop=(k == 7)), reads=[Wq, aT], writes=[bq])
                        evac(qcT.t[:, 4 * j2:4 * j2 + 4, :].rearrange("p k n -> p (k n)"), bq.t[:, :], [bq], [qcT])
                        rel(bq)
                    bs = [bank(), bank()]
                    for hh in range(4):
                        for dc in range(2):
                            P.op(PE, lambda e: e.matmul(bs[hh // 2].t[:, (hh % 2) * 256:(hh % 2 + 1) * 256], lhsT=qcT.t[:, 2 * hh + dc, :],
                                                        rhs=KcT[sq].t[:, 2 * hh + dc, :], start=(dc == 0), stop=(dc == 1)),
                                 reads=[qcT, KcT[sq]], writes=[bs[hh // 2]])
                    S["bs"] = bs

                def s7():
                    bs = S["bs"]
                    for b2_ in range(2):
                        P.op(DVE, lambda e: e.reduce_max(out=mx.t[:, 2 * b2_:2 * b2_ + 2], in_=bs[b2_].t[:, :].rearrange("p (h m) -> p h m", h=2),
                                                         axis=AX.X), reads=[bs[b2_]], writes=[mx])
                    P.op(DVE, lambda e: e.tensor_scalar_mul(out=nmx.t[:], in0=mx.t[:], scalar1=-1.0 / 16.0), reads=[mx], writes=[nmx])
                    for hh in range(4):
                        P.op(ACT, lambda e: e.activation(out=pe_.t[:, hh, :], in_=bs[hh // 2].t[:, (hh % 2) * 256:(hh % 2 + 1) * 256], func=AF.Exp,
                                                         bias=nmx.t[:, hh:hh + 1], scale=1.0 / 16.0, accum_out=ssum.t[:, hh:hh + 1]),
                             reads=[bs[hh // 2], nmx], writes=[pe_, ssum])
                    rel(*bs)
                    P.op(DVE, lambda e: e.reciprocal(out=ssum.t[:], in_=ssum.t[:]), reads=[ssum], writes=[ssum])
                    transpose_tile(pT, lambda b0, nb: pT.t[:, b0:b0 + nb, :], pe_,
                                   lambda b: pe_.t[:, b // 2, (b % 2) * 128:(b % 2 + 1) * 128], 8)

                def s8():
                    bo = [bank(), bank()]
                    for hh in range(4):
                        for mc in range(2):
                            P.op(PE, lambda e: e.matmul(bo[hh // 2].t[:, (hh % 2) * 256:(hh % 2 + 1) * 256], lhsT=pT.t[:, 2 * hh + mc, :],
                                                        rhs=Vc[sq].t[:, mc, hh * 256:(hh + 1) * 256], start=(mc == 0), stop=(mc == 1)),
                                 reads=[pT, Vc[sq]], writes=[bo[hh // 2]])
                    for hh in range(4):
                        P.op(DVE, lambda e: e.tensor_scalar(out=m1.t[:, hh * 256:(hh + 1) * 256], in0=bo[hh // 2].t[:, (hh % 2) * 256:(hh % 2 + 1) * 256],
                                                            scalar1=ssum.t[:, hh:hh + 1], scalar2=None, op0=ALU.mult),
                             reads=[bo[hh // 2], ssum], writes=[m1])
                    rel(*bo)
                    to_T(m1)

                def s9():
                    bc_ = linear2(aT, 8, Woc)
                    P.op(ACT, lambda e: e.activation(out=xin.t[:], in_=tb.t[:], func=AF.Copy, scale=ALPHA), reads=[tb], writes=[xin])
                    for hf in range(2):
                        sl = HS[hf]
                        P.op(DVE, lambda e: e.tensor_tensor(out=xin.t[:, sl], in0=bc_[hf].t[:, :], in1=xin.t[:, sl], op=ALU.add),
                             reads=[bc_[hf], xin], writes=[xin])
                    rel(*bc_)
                    layer_norm(lnt, xin, lnp["ln2_g"], lnp["ln2_b"], X2)
                    P.dma(SP, X2_d[r0:r0 + 128, :], X2.t[:], reads=[X2])

                def s10():
                    transpose_tile(x2T, lambda b0, nb: x2T.t[:, b0:b0 + nb, :], X2, lambda b: X2.t[:, b * 128:(b + 1) * 128], 8)
                    bl_ = bank()
                    for k in range(8):
                        P.op(PE, lambda e: e.matmul(bl_.t[:, 0:NE], lhsT=x2T.t[:, k, :], rhs=wr.t[:, k, :], start=(k == 0), stop=(k == 7)),
                             reads=[x2T, wr], writes=[bl_])
                    P.op(DVE, lambda e: e.tensor_tensor(out=lg.t[:], in0=bl_.t[:, 0:NE], in1=brb.t[:], op=ALU.add), reads=[bl_, brb], writes=[lg])
                    rel(bl_)
                    P.op(DVE, lambda e: e.max(out=top8.t[:], in_=lg.t[:]), reads=[lg], writes=[top8])
                    P.op(DVE, lambda e: e.tensor_scalar(out=msk.t[:], in0=lg.t[:], scalar1=top8.t[:, 3:4], scalar2=None, op0=ALU.is_ge),
                         reads=[lg, top8], writes=[msk])
                    P.op(DVE, lambda e: e.tensor_copy(out=mskb.t[:], in_=msk.t[:]), reads=[msk], writes=[mskb])
                    P.op(DVE, lambda e: e.tensor_scalar_mul(out=nv0.t[:], in0=top8.t[:, 0:1], scalar1=-1.0), reads=[top8], writes=[nv0])
                    P.op(ACT, lambda e: e.activation(out=ex.t[:], in_=lg.t[:], func=AF.Exp, bias=nv0.t[:, 0:1], scale=1.0), reads=[lg, nv0], writes=[ex])
                    P.op(DVE, lambda e: e.tensor_tensor(out=ex.t[:], in0=ex.t[:], in1=msk.t[:], op=ALU.mult), reads=[ex, msk], writes=[ex])
                    P.op(DVE, lambda e: e.reduce_sum(out=esum.t[:], in_=ex.t[:], axis=AX.X), reads=[ex], writes=[esum])
                    P.op(DVE, lambda e: e.reciprocal(out=esum.t[:], in_=esum.t[:]), reads=[esum], writes=[esum])
                    P.op(DVE, lambda e: e.tensor_scalar(out=gate.t[:], in0=ex.t[:], scalar1=esum.t[:, 0:1], scalar2=None, op0=ALU.mult),
                         reads=[ex, esum], writes=[gate])
                    brk = bank()
                    P.op(PE, lambda e: e.matmul(brk.t[:, 0:NE], lhsT=triu.t[:], rhs=mskb.t[:], start=True, stop=True), reads=[triu, mskb], writes=[brk])
                    P.op(PE, lambda e: e.matmul(brk.t[:, 64:64 + NE], lhsT=ones_b.t[:], rhs=mskb.t[:], start=True, stop=True),
                         reads=[ones_b, mskb], writes=[brk])
                    P.op(DVE, lambda e: e.tensor_tensor(out=dest.t[:], in0=brk.t[:, 0:NE], in1=cnt_run.t[:], op=ALU.add), reads=[brk, cnt_run], writes=[dest])
                    P.op(DVE, lambda e: e.tensor_tensor(out=cnt_run.t[:], in0=brk.t[:, 64:64 + NE], in1=cnt_run.t[:], op=ALU.add),
                         reads=[brk, cnt_run], writes=[cnt_run])
                    rel(brk)
                    P.op(DVE, lambda e: e.tensor_scalar(out=oh.t[:], in0=dest.t[:], scalar1=float(CAP) - 0.5, scalar2=None, op0=ALU.is_lt),
                         reads=[dest], writes=[oh])
                    P.op(DVE, lambda e: e.tensor_tensor(out=gate.t[:], in0=gate.t[:], in1=oh.t[:], op=ALU.mult), reads=[gate, oh], writes=[gate])
                    P.op(DVE, lambda e: e.tensor_tensor(out=dest.t[:], in0=dest.t[:], in1=ecap.t[:], op=ALU.add), reads=[dest, ecap], writes=[dest])
                    P.op(DVE, lambda e: e.tensor_scalar(out=dest.t[:], in0=dest.t[:], scalar1=junk.t[:, 0:1], scalar2=None, op0=ALU.subtract),
                         reads=[dest, junk], writes=[dest])
                    P.op(DVE, lambda e: e.tensor_tensor(out=dest.t[:], in0=dest.t[:], in1=oh.t[:], op=ALU.mult), reads=[dest, oh], writes=[dest])
                    P.op(DVE, lambda e: e.tensor_scalar(out=dest.t[:], in0=dest.t[:], scalar1=junk.t[:, 0:1], scalar2=None, op0=ALU.add),
                         reads=[dest, junk], writes=[dest])
                    for k in range(4):
                        P.op(DVE, lambda e: e.tensor_scalar(out=oh.t[:], in0=lg.t[:], scalar1=top8.t[:, k:k + 1], scalar2=None, op0=ALU.is_equal),
                             reads=[lg, top8], writes=[oh])
                        P.op(DVE, lambda e: e.tensor_tensor(out=ohd.t[:], in0=oh.t[:], in1=dest.t[:], op=ALU.mult), reads=[oh, dest], writes=[ohd])
                        P.op(DVE, lambda e: e.reduce_sum(out=dkf.t[:, k:k + 1], in_=ohd.t[:], axis=AX.X), reads=[ohd], writes=[dkf])
                        P.op(DVE, lambda e: e.tensor_tensor(out=ohd.t[:], in0=oh.t[:], in1=gate.t[:], op=ALU.mult), reads=[oh, gate], writes=[ohd])
                        P.op(DVE, lambda e: e.reduce_sum(out=GK.t[:, tt, k:k + 1], in_=ohd.t[:], axis=AX.X), reads=[ohd], writes=[GK])
                    P.op(DVE, lambda e: e.tensor_copy(out=DK.t[:, tt, :], in_=dkf.t[:]), reads=[dkf], writes=[DK])
                    for k in range(4):
                        P.op(POOL, lambda e: e.indirect_dma_start(
                            out=XG_d[:, :], out_offset=bass.IndirectOffsetOnAxis(ap=DK.t[:, tt, k:k + 1], axis=0),
                            in_=X2.t[:], in_offset=None), reads=[X2, DK], dma=True)

                return [s0, s1, s1b, s2, s3, s4, s5, s6, s7, s8, s9, s10]

            strict[0] = True
            for t0_ in range(0, NT, 3):
                grp_ = [make_tile(t_) for t_ in range(t0_, min(NT, t0_ + 3))]
                for k_ in range(len(grp_[0])):
                    for tl_ in grp_:
                        tl_[k_]()
            strict[0] = False
            for i_ in range(8):
                live[i_] = False
            P.dma(SP, cnt_d[:, :], cnt_run.t[:], reads=[cnt_run])
            P.flush()

        if nphase < 5:
            return nc
        with ExitStack() as st:
            A = lambda name, shape, dt=F32: alloc(st, name, shape, dt)
            NBG, NBL = A("NBG", [128, NE, 8]), A("NBL", [128, NE, 8])
            P.dma(SP, NBG.t[:].rearrange("p e c -> p (e c)"), bg_d[:, :], writes=[NBG])
            P.dma(SP, NBL.t[:].rearrange("p e c -> p (e c)"), bl_d[:, :], writes=[NBL])
            for T_ in (NBG, NBL):
                P.op(DVE, lambda e, T_=T_: e.tensor_scalar(out=T_.t[:], in0=T_.t[:], scalar1=-1.0, scalar2=7.0, op0=ALU.mult, op1=ALU.add),
                     reads=[T_], writes=[T_])
            stg = [A("stg5_%d" % i, [128, 2048]) for i in range(3)]
            Wg = [A("Wg%d" % i, [128, 8, 2, 1024], BF16) for i in range(2)]
            Wd = [A("Wd%d" % i, [128, 8, 1024], BF16) for i in range(2)]
            bdb = [A("bdb%d" % i, [128, D]) for i in range(2)]
            xg = [A("xg%d" % i, [128, D]) for i in range(2)]
            XT = [A("XgT%d" % i, [128, 8, CAP], BF16) for i in range(2)]
            actT = A("actT", [128, 8, CAP], BF16)
            rg = [A("rg%d" % i, [128, 512]) for i in range(2)]
            sv = [A("sv%d" % i, [128, 512]) for i in range(2)]
            rl = [A("rl%d" % i, [128, 512]) for i in range(2)]
            yt = [A("yt%d" % i, [128, D]) for i in range(2)]
            stg_rr = [0]

            def weight_chunks(ex_):
                s_ = ex_ % 2
                WG, WD, BD = Wg[s_], Wd[s_], bdb[s_]
                th = []
                for k in range(8):
                    def f(k=k):
                        sg = stg[stg_rr[0] % 3]
                        stg_rr[0] += 1
                        P.dma(SP, sg.t[:], wgu_d[ex_, k * 128:(k + 1) * 128, :], writes=[sg])
                        evac(WG.t[:, k, :, :], sg.t[:].rearrange("p (f two) -> p two f", two=2), [sg], [WG], engs=(ACT, DVE))
                    th.append(f)
                for k2 in range(4):
                    def f(k2=k2):
                        sg = stg[stg_rr[0] % 3]
                        stg_rr[0] += 1
                        P.dma(SP, sg.t[:].rearrange("p (a n) -> p a n", a=2),
                              wd_d[ex_, k2 * 256:(k2 + 1) * 256, :].rearrange("(a p) n -> p a n", p=128), writes=[sg])
                        evac(WD.t[:, 2 * k2:2 * k2 + 2, :], sg.t[:].rearrange("p (a n) -> p a n", a=2), [sg], [WD], engs=(ACT, DVE))
                    th.append(f)
                th.append(lambda: P.dma(SP, BD.t[:], bd_d[ex_, :].partition_broadcast(128), writes=[BD]))
                return th

            def prep_x(ex_):
                XGT = XT[ex_ % 2]
                for stl in range(CAP // 128):
                    XGt = xg[stl % 2]
                    P.dma(SP, XGt.t[:], XG_d[ex_ * CAP + stl * 128: ex_ * CAP + (stl + 1) * 128, :], writes=[XGt])
                    transpose_tile(XGT, lambda b0, nb, stl=stl: XGT.t[:, b0:b0 + nb, stl * 128:(stl + 1) * 128], XGt,
                                   lambda b, XGt=XGt: XGt.t[:, b * 128:(b + 1) * 128], 8)

            for f in weight_chunks(0):
                f()
            prep_x(0)
            for ex_ in range(NE):
                s_ = ex_ % 2
                WG, WD, BD, XGT = Wg[s_], Wd[s_], bdb[s_], XT[s_]
                pend = weight_chunks(ex_ + 1) if ex_ + 1 < NE else []
                for (c0, cw) in ((0, 512), (512, CAP - 512)):
                    for fc in range(8):
                        i2 = fc % 2
                        bg_, bl2 = bank(), bank()
                        for k in range(8):
                            P.op(PE, lambda e, k=k: e.matmul(bg_.t[:, 0:cw], lhsT=WG.t[:, k, 0, fc * 128:(fc + 1) * 128], rhs=XGT.t[:, k, c0:c0 + cw],
                                                            start=(k == 0), stop=(k == 7)), reads=[WG, XGT], writes=[bg_])
                        for k in range(8):
                            P.op(PE, lambda e, k=k: e.matmul(bl2.t[:, 0:cw], lhsT=WG.t[:, k, 1, fc * 128:(fc + 1) * 128], rhs=XGT.t[:, k, c0:c0 + cw],
                                                            start=(k == 0), stop=(k == 7)), reads=[WG, XGT], writes=[bl2])
                        RG, SV, RL = rg[i2], sv[i2], rl[i2]
                        P.op(ACT, lambda e: e.activation(out=RG.t[:, 0:cw], in_=bg_.t[:, 0:cw], func=AF.Relu, bias=NBG.t[:, ex_, fc:fc + 1], scale=-1.0),
                             reads=[bg_, NBG], writes=[RG])
                        P.op(ACT, lambda e: e.activation(out=SV.t[:, 0:cw], in_=RG.t[:, 0:cw], func=AF.Silu, bias=nb7.t[:, 0:1], scale=-1.702),
                             reads=[RG, nb7], writes=[SV])
                        P.op(ACT, lambda e: e.activation(out=RL.t[:, 0:cw], in_=bl2.t[:, 0:cw], func=AF.Relu, bias=NBL.t[:, ex_, fc:fc + 1], scale=-1.0),
                             reads=[bl2, NBL], writes=[RL])
                        P.op(DVE, lambda e: e.tensor_scalar(out=RL.t[:, 0:cw], in0=RL.t[:, 0:cw], scalar1=14.0, scalar2=-8.0, op0=ALU.min, op1=ALU.add),
                             reads=[RL], writes=[RL])
                        P.op(DVE, lambda e: e.scalar_tensor_tensor(out=actT.t[:, fc, c0:c0 + cw], in0=RL.t[:, 0:cw], scalar=-1.0, in1=SV.t[:, 0:cw],
                                                                   op0=ALU.mult, op1=ALU.mult), reads=[RL, SV], writes=[actT])
                        if pend:
                            pend.pop(0)()
                if ex_ + 1 < NE:
                    prep_x(ex_ + 1)
                for stl in range(CAP // 128):
                    YT = yt[stl % 2]
                    for hf in range(2):
                        bk = bank()
                        for fc in range(8):
                            P.op(PE, lambda e, fc=fc: e.matmul(bk.t[:, :], lhsT=actT.t[:, fc, stl * 128:(stl + 1) * 128], rhs=WD.t[:, fc, hf * 512:(hf + 1) * 512],
                                                              start=(fc == 0), stop=(fc == 7)), reads=[actT, WD], writes=[bk])
                        P.op(DVE, lambda e: e.scalar_tensor_tensor(out=YT.t[:, hf * 512:(hf + 1) * 512], in0=bk.t[:, :], scalar=1.0 / 1.702,
                                                                   in1=BD.t[:, hf * 512:(hf + 1) * 512], op0=ALU.mult, op1=ALU.add),
                             reads=[bk, BD], writes=[YT])
                    P.dma(POOL, YG_d[ex_ * CAP + stl * 128: ex_ * CAP + (stl + 1) * 128, :], YT.t[:], reads=[YT])
                    if pend:
                        pend.pop(0)()
                while pend:
                    pend.pop(0)()
            P.flush()

        if nphase < 6:
            return nc
        with ExitStack() as st:
            A = lambda name, shape, dt=F32: alloc(st, name, shape, dt)
            g3, b3 = A("g3", [128, D]), A("b3", [128, D])
            P.dma(SP, g3.t[:], ln_d["ln3_g"][0, :].partition_broadcast(128), writes=[g3])
            P.dma(SP, b3.t[:], ln_d["ln3_b"][0, :].partition_broadcast(128), writes=[b3])
            x2t = [A("c_x2_%d" % i, [128, D]) for i in range(2)]
            yk = [[A("c_y%d_%d" % (i, k), [128, D]) for k in range(4)] for i in range(2)]
            acc = [A("c_acc%d" % i, [128, D]) for i in range(2)]
            res = [A("c_res%d" % i, [128, D]) for i in range(2)]
            stats, mv, rstd = A("c_stats", [128, 2, 6]), A("c_mv", [128, 2]), A("c_rstd", [128, 1])
            for tt in range(NT):
                s = tt % 2
                r0 = tt * 128
                X2, ACC, RES = x2t[s], acc[s], res[s]
                P.dma(SP, X2.t[:], X2_d[r0:r0 + 128, :], writes=[X2])
                for k in range(4):
                    P.op(POOL, lambda e, k=k, tt=tt, s=s: e.indirect_dma_start(
                        out=yk[s][k].t[:], out_offset=None, in_=YG_d[:, :],
                        in_offset=bass.IndirectOffsetOnAxis(ap=DK.t[:, tt, k:k + 1], axis=0)), reads=[DK], writes=[yk[s][k]], dma=True)
                P.op(ACT, lambda e, X2=X2, ACC=ACC: e.activation(out=ACC.t[:], in_=X2.t[:], func=AF.Copy, scale=ALPHA), reads=[X2], writes=[ACC])
                for k in range(4):
                    P.op(DVE, lambda e, k=k, tt=tt, s=s, ACC=ACC: e.scalar_tensor_tensor(
                        out=ACC.t[:], in0=yk[s][k].t[:], scalar=GK.t[:, tt, k:k + 1], in1=ACC.t[:], op0=ALU.mult, op1=ALU.add),
                        reads=[yk[s][k], GK, ACC], writes=[ACC])
                layer_norm((stats, mv, rstd), ACC, g3, b3, RES)
                P.dma(SP, out_d[r0:r0 + 128, :], RES.t[:], reads=[RES])
            P.flush()
    except _Stop:
        pass
    return nc


def _core_inputs(inp, c):
    f = lambda a: np.ascontiguousarray(a, dtype=np.float32)
    b0 = c * NSEQ
    m = {}
    m["x"] = f(inp["x"][b0:b0 + NSEQ].reshape(TOK, D))
    m["mem"] = f(inp["mem"][b0:b0 + NSEQ].reshape(NSEQ * 256, D))
    pos = np.asarray(inp["positions"][b0:b0 + NSEQ]).reshape(NT, 128).astype(np.int32)
    m["pos_t"] = np.ascontiguousarray(pos.T)
    return m


def _shared_inputs(inp):
    f = lambda a: np.ascontiguousarray(a, dtype=np.float32)
    m = {}
    for k in ("w_in", "w_attn_o", "w_glu_a", "w_glu_b", "w_out", "wq_c", "wk_c", "wv_c", "wo_c", "w_router",
              "w_gate_up", "w_down", "b_down"):
        m[k] = f(np.asarray(inp[k])[0])
    for k in ("sinks", "ln1_g", "ln1_b", "ln2_g", "ln2_b", "ln3_g", "ln3_b", "b_router"):
        m[k] = f(np.asarray(inp[k]).reshape(1, -1))
    lr = np.asarray(inp["lam_re"])[0]
    li = np.asarray(inp["lam_im"])[0]
    m["lamre_t"] = f(np.concatenate([lr.T, lr.T], 0))
    m["lamim_t"] = f(np.concatenate([li.T, li.T], 0))
    m["logdt_t"] = f(np.broadcast_to(np.asarray(inp["log_dt"])[0][None, :], (128, 32)))
    bre = np.asarray(inp["b_re"])[0].transpose(1, 0, 2)
    bim = np.asarray(inp["b_im"])[0].transpose(1, 0, 2)
    m["bcat"] = f(np.concatenate([bre, bim], 0).reshape(128, 512))
    m["bsw"] = f(np.concatenate([bim, bre], 0).reshape(128, 512))
    cre = np.asarray(inp["c_re"])[0].transpose(2, 0, 1)
    cim = np.asarray(inp["c_im"])[0].transpose(2, 0, 1)
    m["ca"] = f(np.concatenate([cre, cim], 0).reshape(128, 512))
    m["cb"] = f(np.concatenate([cim, cre], 0).reshape(128, 512))
    m["dskip_t"] = f(np.asarray(inp["d_skip"])[0].reshape(4, 128).T)
    bgu = np.asarray(inp["b_gate_up"])[0]
    bg = bgu[:, 0::2].reshape(NE, 8, 128).transpose(2, 0, 1)
    bl = bgu[:, 1::2].reshape(NE, 8, 128).transpose(2, 0, 1)
    m["bg_t"] = f(bg.reshape(128, NE * 8))
    m["bl_t"] = f(bl.reshape(128, NE * 8))
    return m


_NC_CACHE = {}


def kernel(**inputs):
    if "nc" not in _NC_CACHE:
        _NC_CACHE["nc"] = build_nc()
    nc = _NC_CACHE["nc"]
    shared = _shared_inputs(inputs)
    in_maps = []
    for c in range(NCORES):
        m = dict(shared)
        m.update(_core_inputs(inputs, c))
        in_maps.append(m)
    res = run_bass_kernel_spmd(nc, in_maps, core_ids=list(range(NCORES)))
    outs = [np.asarray(r["out"]).reshape(NSEQ, SEQ, D) for r in res.results]
    return np.concatenate(outs, 0).astype(np.float32)
```

```python
import math
import numpy as np
from contextlib import ExitStack
import concourse.bass as bass
import concourse.mybir as mybir
from concourse.bass_utils import run_bass_kernel_spmd

F32 = mybir.dt.float32
BF16 = mybir.dt.bfloat16
I32 = mybir.dt.int32
U32 = mybir.dt.uint32
AF = mybir.ActivationFunctionType
ALU = mybir.AluOpType
AX = mybir.AxisListType

PE, ACT, DVE, POOL, SP = "pe", "act", "dve", "pool", "sp"

NCORES = 8
D = 1024
SEQ = 2048
NSEQ = 2
NT = NSEQ * SEQ // 128
TOK = NSEQ * SEQ
INW = 3840
NE = 32
CAP = 768
NSLOT = NE * CAP
ALPHA = 2.0 ** 0.25
LN_EPS = 1e-5
PI = math.pi


class Buf:
    __slots__ = ("name", "last_w", "readers")

    def __init__(self, name):
        self.name = name
        self.last_w = None
        self.readers = []


class Op:
    __slots__ = ("eng", "fn", "reads", "writes", "deps", "signal", "cnt", "is_dma", "ring", "ringval")

    def __init__(self, eng, fn, reads, writes, is_dma):
        self.eng = eng
        self.fn = fn
        self.reads = reads
        self.writes = writes
        self.deps = []
        self.signal = False
        self.cnt = None
        self.is_dma = is_dma
        self.ring = None
        self.ringval = None


class _Stop(Exception):
    pass


class _Rec:
    def __init__(self):
        self.calls = []

    def __getattr__(self, name):
        def f(*args, **kwargs):
            self.calls.append((name, args, kwargs))
            return None
        return f


class TB:
    __slots__ = ("t", "b")

    def __init__(self, t, b):
        self.t = t
        self.b = b


class Prog:
    NRING = 8

    def __init__(self, nc, stack):
        self.nc = nc
        self.ops = []
        self.engs = {PE: nc.tensor, ACT: nc.scalar, DVE: nc.vector, POOL: nc.gpsimd, SP: nc.sync}
        self.sem = {e: stack.enter_context(nc.semaphore("s_" + e)) for e in (PE, ACT, DVE, POOL)}
        self.rings = {e: [stack.enter_context(nc.semaphore("r_%s%d" % (e, i))) for i in range(self.NRING)]
                      for e in (SP, POOL, ACT)}
        self.cnt = {e: 0 for e in (PE, ACT, DVE, POOL)}
        self.dma_n = {e: 0 for e in (SP, POOL, ACT)}
        self.ring_issued = {e: [0] * self.NRING for e in (SP, POOL, ACT)}
        self.known = {e: {} for e in (PE, ACT, DVE, POOL, SP)}
        self.bufs = []
        self.rr = 0

    def buf(self, name):
        b = Buf(name)
        self.bufs.append(b)
        return b

    def op(self, eng, fn, reads=(), writes=(), dma=False):
        rec = _Rec()
        fn(rec)
        assert len(rec.calls) == 1
        o = Op(eng, rec.calls[0], [r.b if isinstance(r, TB) else r for r in reads],
               [w.b if isinstance(w, TB) else w for w in writes], dma)
        self.ops.append(o)
        return o

    def dma(self, eng, out, in_, reads=(), writes=(), **kw):
        return self.op(eng, lambda e: e.dma_start(out=out, in_=in_, **kw), reads, writes, dma=True)

    def flush(self):
        ops = self.ops
        self.ops = []
        last = {}
        for o in ops:
            if not o.is_dma:
                last[o.eng] = o
        for o in last.values():
            o.signal = True
        for o in ops:
            deps = []
            for b in o.reads:
                if b.last_w is not None:
                    deps.append(b.last_w)
            for b in o.writes:
                if b.last_w is not None:
                    deps.append(b.last_w)
                deps.extend(b.readers)
            seen = set()
            for d in deps:
                if d is o or id(d) in seen:
                    continue
                seen.add(id(d))
                if d.eng == PE and o.eng == PE and not d.is_dma and not o.is_dma:
                    continue
                o.deps.append(d)
                d.signal = True
            for b in o.reads:
                b.readers.append(o)
            for b in o.writes:
                b.last_w = o
                b.readers = []
        for o in ops:
            eng = self.engs[o.eng]
            kn = self.known[o.eng]
            need = {}
            for d in o.deps:
                if d.is_dma:
                    key = ("r", d.eng, d.ring)
                    need[key] = max(need.get(key, 0), d.ringval)
                else:
                    key = ("c", d.eng)
                    need[key] = max(need.get(key, 0), d.cnt)
            if o.is_dma:
                ring = self.dma_n[o.eng] % self.NRING
                prev = self.ring_issued[o.eng][ring]
                if prev > 0:
                    key = ("r", o.eng, ring)
                    need[key] = max(need.get(key, 0), prev)
            for key, val in need.items():
                if kn.get(key, 0) >= val:
                    continue
                kn[key] = val
                if key[0] == "c":
                    eng.wait_ge(self.sem[key[1]], val)
                else:
                    eng.wait_ge(self.rings[key[1]][key[2]], val)
            name_, args_, kwargs_ = o.fn
            ins = getattr(eng, name_)(*args_, **kwargs_)
            if o.is_dma:
                n = self.dma_n[o.eng]
                ring = n % self.NRING
                self.dma_n[o.eng] = n + 1
                val = self.ring_issued[o.eng][ring] + 16
                self.ring_issued[o.eng][ring] = val
                o.ring = ring
                o.ringval = val
                ins.then_inc(self.rings[o.eng][ring], 16)
            elif o.signal:
                self.cnt[o.eng] += 1
                o.cnt = self.cnt[o.eng]
                ins.then_inc(self.sem[o.eng], 1)
            o.fn = None
        for en, eng in self.engs.items():
            kn = self.known[en]
            for ce in (PE, ACT, DVE, POOL):
                v = self.cnt[ce]
                if ce != en and v > kn.get(("c", ce), 0):
                    kn[("c", ce)] = v
                    eng.wait_ge(self.sem[ce], v)
            for qe in self.rings:
                for r in range(self.NRING):
                    v = self.ring_issued[qe][r]
                    if v > kn.get(("r", qe, r), 0):
                        kn[("r", qe, r)] = v
                        eng.wait_ge(self.rings[qe][r], v)
        for b in self.bufs:
            b.last_w = None
            b.readers = []


def build_nc(debug=False, nphase=9, dbg3=0, skip=(), dbgt=0):
    nc = bass.Bass("TRN2", target_bir_lowering=False)

    def din(name, shape, dt=F32):
        return nc.dram_tensor(name, list(shape), dt, kind="ExternalInput").ap()

    def dscr(name, shape, dt=F32):
        return nc.dram_tensor(name, list(shape), dt, kind=("ExternalOutput" if debug else "Internal")).ap()

    x_d = din("x", [TOK, D])
    mem_d = din("mem", [NSEQ * 256, D])
    pos_d = din("pos_t", [128, NT], I32)
    w_in_d = din("w_in", [D, INW])
    sinks_d = din("sinks", [1, 16])
    w_attn_o_d = din("w_attn_o", [D, D])
    lr_d = din("lamre_t", [128, 32])
    li_d = din("lamim_t", [128, 32])
    ldt_d = din("logdt_t", [128, 32])
    bcat_d = din("bcat", [128, 32 * 16])
    bsw_d = din("bsw", [128, 32 * 16])
    ca_d = din("ca", [128, 32 * 16])
    cb_d = din("cb", [128, 32 * 16])
    dsk_d = din("dskip_t", [128, 4])
    w_glu_a_d = din("w_glu_a", [512, D])
    w_glu_b_d = din("w_glu_b", [512, D])
    w_out_d = din("w_out", [D, D])
    ln_d = {k: din(k, [1, D]) for k in ("ln1_g", "ln1_b", "ln2_g", "ln2_b", "ln3_g", "ln3_b")}
    wq_d = din("wq_c", [D, D])
    wk_d = din("wk_c", [D, D])
    wv_d = din("wv_c", [D, D])
    wo_d = din("wo_c", [D, D])
    wr_d = din("w_router", [D, NE])
    br_d = din("b_router", [1, NE])
    wgu_d = din("w_gate_up", [NE, D, 2 * D])
    bg_d = din("bg_t", [128, NE * 8])
    bl_d = din("bl_t", [128, NE * 8])
    wd_d = din("w_down", [NE, D, D])
    bd_d = din("b_down", [NE, D])
    out_d = nc.dram_tensor("out", [TOK, D], F32, kind="ExternalOutput").ap()
    cnt_d = nc.dram_tensor("cnt_out", [128, NE], F32, kind="ExternalOutput").ap()

    QK_d = dscr("s_qk", [TOK, 1152])
    V_d = dscr("s_v", [TOK, 128])
    U_d = dscr("s_u", [TOK, 512])
    G_d = nc.dram_tensor("s_g", [TOK, 2048], BF16, kind="Internal").ap()
    O_d = dscr("s_o", [TOK, D])
    ZT_d = nc.dram_tensor("s_zt", [128, 4, TOK], BF16, kind="Internal").ap()
    YDBG_d = dscr("s_ydbg", [128, 4, TOK]) if debug else None
    X2_d = dscr("s_x2", [TOK, D])
    XG_d = dscr("s_xg", [NSLOT + 128, D])
    YG_d = dscr("s_yg", [NSLOT + 128, D])

    try:
      with ExitStack() as top:
        P = Prog(nc, top)

        cur_tt = [0]

        def chk(i):
            if dbg3 >= 10 and dbg3 - 10 == i and cur_tt[0] == dbgt:
                P.flush()
                raise _Stop()

        def alloc(st, name, shape, dt):
            return TB(st.enter_context(nc.sbuf_tensor("sb_" + name, list(shape), dt)), P.buf(name))

        banks = [TB(top.enter_context(nc.psum_tensor("bank%d" % i, [128, 512], F32)), P.buf("bank%d" % i))
                 for i in range(8)]
        bank_sets = {"all": list(range(8)), "h": [0, 1, 2, 3, 4, 5], "t": [6, 7]}
        bank_ctr = {"all": 0, "h": 0, "t": 0}
        cur_set = ["all"]

        strict = [False]
        live = [False] * 8

        def bank():
            k_ = cur_set[0]
            lst = bank_sets[k_]
            idx = lst[bank_ctr[k_] % len(lst)]
            bank_ctr[k_] += 1
            if strict[0]:
                tries = 0
                while live[idx]:
                    idx = lst[bank_ctr[k_] % len(lst)]
                    bank_ctr[k_] += 1
                    tries += 1
                    assert tries <= len(lst), "all PSUM banks hold results whose consumers are not yet emitted"
                live[idx] = True
            return banks[idx]

        def rel(*bks):
            for b_ in bks:
                live[banks.index(b_)] = False

        ev_rr = [0]

        def evac(out_ap, in_ap, reads, writes, scale=None, engs=(DVE, ACT)):
            e = engs[ev_rr[0] % len(engs)]
            ev_rr[0] += 1
            if e == ACT:
                P.op(ACT, lambda en: en.activation(out=out_ap, in_=in_ap, func=AF.Copy, scale=(1.0 if scale is None else scale)), reads, writes)
            else:
                engn = e
                if scale is None:
                    P.op(engn, lambda en: en.tensor_copy(out=out_ap, in_=in_ap), reads, writes)
                else:
                    P.op(engn, lambda en: en.tensor_scalar_mul(out=out_ap, in0=in_ap, scalar1=scale), reads, writes)

        ident = alloc(top, "ident", [128, 128], F32)
        P.op(POOL, lambda e: e.memset(ident.t[:], 1.0), writes=[ident])
        P.op(POOL, lambda e: e.affine_select(out=ident.t[:], in_=ident.t[:], pattern=[[-1, 128]],
                                             compare_op=ALU.is_equal, fill=0.0, base=0, channel_multiplier=1),
             reads=[ident], writes=[ident])
        nb7 = alloc(top, "nb7", [128, 1], F32)
        P.op(POOL, lambda e: e.memset(nb7.t[:], 1.702 * 7.0), writes=[nb7])
        GK = alloc(top, "GK", [128, NT, 4], F32)
        DK = alloc(top, "DK", [128, NT, 4], I32)
        P.flush()

        def transpose_tile(dst, dst_ap_fn, src, src_ap_fn, nblk, extra_reads=()):
            b0 = 0
            while b0 < nblk:
                nb = min(4, nblk - b0)
                bk = bank()
                for j in range(nb):
                    P.op(PE, lambda e, bk=bk, j=j, b=b0 + j: e.transpose(
                        out=bk.t[:, j * 128:(j + 1) * 128], in_=src_ap_fn(b), identity=ident.t[:]),
                        reads=[src, ident] + list(extra_reads), writes=[bk])
                evac(dst_ap_fn(b0, nb), bk.t[:, 0:nb * 128].rearrange("p (k n) -> p k n", k=nb), [bk], [dst])
                rel(bk)
                b0 += nb

        def load_weight_bf16(st, name, w_ap, K, N, stage):
            nk = K // 128
            W = alloc(st, name, [128, nk, N], BF16)
            for k in range(nk):
                sg = stage[k % len(stage)]
                P.dma(SP, sg.t[:, 0:N], w_ap[k * 128:(k + 1) * 128, :], writes=[sg])
                evac(W.t[:, k, :], sg.t[:, 0:N], [sg], [W], engs=(DVE, ACT, POOL))
            return W

        def fill_weight(W, w_ap, K, N, stage):
            for k in range(K // 128):
                sg = stage[k % len(stage)]
                P.dma(SP, sg.t[:, 0:N], w_ap[k * 128:(k + 1) * 128, :], writes=[sg])
                evac(W.t[:, k, :], sg.t[:, 0:N], [sg], [W], engs=(DVE, ACT))

        def layer_norm(st_tiles, xin, g_bc, b_bc, out_tb):
            stats, mv, rstd = st_tiles
            for c in range(2):
                P.op(DVE, lambda e, c=c: e.bn_stats(out=stats.t[:, c, :], in_=xin.t[:, c * 512:(c + 1) * 512]),
                     reads=[xin], writes=[stats])
            P.op(DVE, lambda e: e.bn_aggr(out=mv.t[:], in_=stats.t[:]), reads=[stats], writes=[mv])
            P.op(DVE, lambda e: e.tensor_scalar_add(out=rstd.t[:], in0=mv.t[:, 1:2], scalar1=LN_EPS), reads=[mv], writes=[rstd])
            P.op(ACT, lambda e: e.activation(out=rstd.t[:], in_=rstd.t[:], func=AF.Ln), reads=[rstd], writes=[rstd])
            P.op(ACT, lambda e: e.activation(out=rstd.t[:], in_=rstd.t[:], func=AF.Exp, scale=-0.5), reads=[rstd], writes=[rstd])
            P.op(DVE, lambda e: e.tensor_scalar(out=out_tb.t[:], in0=xin.t[:], scalar1=mv.t[:, 0:1],
                                                scalar2=rstd.t[:, 0:1], op0=ALU.subtract, op1=ALU.mult),
                 reads=[xin, mv, rstd], writes=[out_tb])
            P.op(DVE, lambda e: e.tensor_tensor(out=out_tb.t[:], in0=out_tb.t[:], in1=g_bc.t[:], op=ALU.mult),
                 reads=[out_tb, g_bc], writes=[out_tb])
            P.op(DVE, lambda e: e.tensor_tensor(out=out_tb.t[:], in0=out_tb.t[:], in1=b_bc.t[:], op=ALU.add),
                 reads=[out_tb, b_bc], writes=[out_tb])

        sr_n = [0]

        def sin_reduced(st_tmp, out_ap, ang_ap, shift, reads, writes, tmp):
            shp = list(ang_ap.shape)
            sr_n[0] += 1
            ki = alloc(st_tmp, "sr_ki%d" % sr_n[0], shp, I32)
            kf = alloc(st_tmp, "sr_kf%d" % sr_n[0], shp, F32)
            xs = alloc(st_tmp, "sr_xs%d" % sr_n[0], shp, F32)
            P.op(DVE, lambda e: e.tensor_scalar_add(out=xs.t[:], in0=ang_ap, scalar1=shift + 2 * PI), reads=reads, writes=[xs])
            P.op(DVE, lambda e: e.tensor_scalar_mul(out=kf.t[:], in0=xs.t[:], scalar1=1.0 / (2 * PI)), reads=[xs], writes=[kf])
            P.op(DVE, lambda e: e.tensor_copy(out=ki.t[:], in_=kf.t[:]), reads=[kf], writes=[ki])
            P.op(DVE, lambda e: e.tensor_copy(out=kf.t[:], in_=ki.t[:]), reads=[ki], writes=[kf])
            P.op(DVE, lambda e: e.scalar_tensor_tensor(out=xs.t[:], in0=kf.t[:], scalar=-2 * PI, in1=xs.t[:], op0=ALU.mult, op1=ALU.add),
                 reads=[kf, xs], writes=[xs])
            P.op(DVE, lambda e: e.tensor_scalar(out=kf.t[:], in0=xs.t[:], scalar1=PI, scalar2=-2 * PI, op0=ALU.is_gt, op1=ALU.mult),
                 reads=[xs], writes=[kf])
            P.op(DVE, lambda e: e.tensor_tensor(out=xs.t[:], in0=xs.t[:], in1=kf.t[:], op=ALU.add), reads=[xs, kf], writes=[xs])
            P.op(DVE, lambda e: e.tensor_scalar(out=kf.t[:], in0=xs.t[:], scalar1=-PI, scalar2=2 * PI, op0=ALU.is_lt, op1=ALU.mult),
                 reads=[xs], writes=[kf])
            P.op(DVE, lambda e: e.tensor_tensor(out=xs.t[:], in0=xs.t[:], in1=kf.t[:], op=ALU.add), reads=[xs, kf], writes=[xs])
            P.op(DVE, lambda e: e.tensor_scalar(out=xs.t[:], in0=xs.t[:], scalar1=-PI + 1e-5, scalar2=PI - 1e-5, op0=ALU.max, op1=ALU.min),
                 reads=[xs], writes=[xs])
            P.op(ACT, lambda e: e.activation(out=out_ap, in_=xs.t[:], func=AF.Sin), reads=[xs], writes=writes)

        with ExitStack() as st:
          if 1 not in skip:
            cosr = alloc(st, "cosr", [128, NT, 32], F32)
            sinr = alloc(st, "sinr", [128, NT, 32], F32)
            Win = alloc(st, "Win", [128, 8, INW], BF16)
            with ExitStack() as st1:
                stage = [alloc(st1, "stg0", [128, INW], F32)]
                posi = alloc(st1, "posi", [128, NT], I32)
                posf = alloc(st1, "posf", [128, NT], F32)
                invf = alloc(st1, "invf", [128, 32], F32)
                iot = alloc(st1, "iot", [128, 32], I32)
                ang = alloc(st1, "ang", [128, NT, 32], F32)
                P.dma(SP, posi.t[:], pos_d[:, :], writes=[posi])
                P.op(DVE, lambda e: e.tensor_copy(out=posf.t[:], in_=posi.t[:]), reads=[posi], writes=[posf])
                P.op(POOL, lambda e: e.iota(iot.t[:], pattern=[[1, 32]], base=0, channel_multiplier=0), writes=[iot])
                P.op(DVE, lambda e: e.tensor_copy(out=invf.t[:], in_=iot.t[:]), reads=[iot], writes=[invf])
                P.op(ACT, lambda e: e.activation(out=invf.t[:], in_=invf.t[:], func=AF.Exp,
                                                 scale=-math.log(10000.0) / 32.0), reads=[invf], writes=[invf])
                P.op(DVE, lambda e: e.tensor_tensor(out=ang.t[:], in0=posf.t[:].unsqueeze(2).broadcast_to([128, NT, 32]),
                                                    in1=invf.t[:].unsqueeze(1).broadcast_to([128, NT, 32]), op=ALU.mult),
                     reads=[posf, invf], writes=[ang])
                sin_reduced(st1, cosr.t[:], ang.t[:], PI / 2, [ang], [cosr], None)
                sin_reduced(st1, sinr.t[:], ang.t[:], 0.0, [ang], [sinr], None)
                fill_weight(Win, w_in_d, D, INW, stage)
                P.flush()
            xt = [alloc(st, "xt%d" % i, [128, D], F32) for i in range(2)]
            xT = [alloc(st, "xT%d" % i, [128, 8, 128], BF16) for i in range(2)]
            pj = [alloc(st, "pj%d" % i, [128, INW], F32) for i in range(2)]
            pjb = [[TB(pj[i].t, P.buf("pj%d_%d" % (i, cb))) for cb in range(8)] for i in range(2)]
            qk = [alloc(st, "qk%d" % i, [128, 1152], F32) for i in range(2)]
            gt = [alloc(st, "gt%d" % i, [128, 2048], BF16) for i in range(2)]
            ra = alloc(st, "ra", [128, 576], F32)
            rb = alloc(st, "rb", [128, 576], F32)
            rc = alloc(st, "rc", [128, 576], F32)
            rd = alloc(st, "rd", [128, 576], F32)

            def p1_front(tt):
                s = tt % 2
                X, XT, PJ = xt[s], xT[s], pj[s]
                r0 = tt * 128
                P.dma(SP, X.t[:], x_d[r0:r0 + 128, :], writes=[X])
                transpose_tile(XT, lambda b0, nb: XT.t[:, b0:b0 + nb, :], X, lambda b: X.t[:, b * 128:(b + 1) * 128], 8)
                for cb in range(8):
                    c0 = cb * 512
                    cw = min(512, INW - c0)
                    bk = bank()
                    for k in range(8):
                        P.op(PE, lambda e: e.matmul(bk.t[:, 0:cw], lhsT=XT.t[:, k, :], rhs=Win.t[:, k, c0:c0 + cw],
                                                    start=(k == 0), stop=(k == 7)), reads=[XT, Win], writes=[bk])
                    evac(PJ.t[:, c0:c0 + cw], bk.t[:, 0:cw], [bk], [pjb[s][cb]])

            def p1_back(tt):
                s = tt % 2
                PJ, QK, GT, B_ = pj[s], qk[s], gt[s], pjb[s]
                r0 = tt * 128
                pv = PJ.t[:, 0:1152].rearrange("p (h t f) -> p h t f", h=18, t=2)
                qv = QK.t[:].rearrange("p (h t f) -> p h t f", h=18, t=2)
                cb_ = cosr.t[:, tt, :].unsqueeze(1).broadcast_to([128, 18, 32])
                sb_ = sinr.t[:, tt, :].unsqueeze(1).broadcast_to([128, 18, 32])
                r3 = lambda T_: T_.t[:].rearrange("p (h f) -> p h f", h=18)
                rq = [B_[0], B_[1], B_[2]]
                P.op(DVE, lambda e: e.tensor_tensor(out=r3(ra), in0=pv[:, :, 0, :], in1=cb_, op=ALU.mult), reads=rq + [cosr], writes=[ra])
                P.op(POOL, lambda e: e.tensor_tensor(out=r3(rb), in0=pv[:, :, 1, :], in1=sb_, op=ALU.mult), reads=rq + [sinr], writes=[rb])
                P.op(DVE, lambda e: e.tensor_tensor(out=qv[:, :, 0, :], in0=r3(ra), in1=r3(rb), op=ALU.subtract), reads=[ra, rb], writes=[QK])
                P.op(POOL, lambda e: e.tensor_tensor(out=r3(rc), in0=pv[:, :, 1, :], in1=cb_, op=ALU.mult), reads=rq + [cosr], writes=[rc])
                P.op(DVE, lambda e: e.tensor_tensor(out=r3(rd), in0=pv[:, :, 0, :], in1=sb_, op=ALU.mult), reads=rq + [sinr], writes=[rd])
                P.op(POOL, lambda e: e.tensor_tensor(out=qv[:, :, 1, :], in0=r3(rc), in1=r3(rd), op=ALU.add), reads=[rc, rd], writes=[QK])
                P.op(ACT, lambda e: e.activation(out=GT.t[:], in_=PJ.t[:, 1792:3840], func=AF.Tanh, scale=0.5), reads=B_[3:8], writes=[GT])
                P.dma(SP, U_d[r0:r0 + 128, :], PJ.t[:, 1280:1792], reads=[B_[2], B_[3]])
                P.dma(SP, G_d[r0:r0 + 128, :], GT.t[:], reads=[GT])

            mprev = alloc(st, "mprev", [128, 128], BF16)
            mcur = alloc(st, "mcur", [128, 128], BF16)
            P.op(POOL, lambda e: e.memset(mprev.t[:], 1.0), writes=[mprev])
            P.op(POOL, lambda e: e.affine_select(out=mprev.t[:], in_=mprev.t[:], pattern=[[-1, 128]],
                                                 compare_op=ALU.is_gt, fill=0.0, base=0, channel_multiplier=1),
                 reads=[mprev], writes=[mprev])
            P.op(POOL, lambda e: e.memset(mcur.t[:], 1.0), writes=[mcur])
            P.op(POOL, lambda e: e.affine_select(out=mcur.t[:], in_=mcur.t[:], pattern=[[1, 128]],
                                                 compare_op=ALU.is_ge, fill=0.0, base=0, channel_multiplier=-1),
                 reads=[mcur], writes=[mcur])
            esink = alloc(st, "esink", [128, 16], F32)
            P.dma(SP, esink.t[:], sinks_d[0, :].partition_broadcast(128), writes=[esink])
            P.op(ACT, lambda e: e.activation(out=esink.t[:], in_=esink.t[:], func=AF.Exp), reads=[esink], writes=[esink])
            qT = [alloc(st, "a_qT%d" % i, [64, 16, 128], BF16) for i in range(2)]
            kT = [alloc(st, "a_kT%d" % i, [64, 2, 128], BF16) for i in range(3)]
            va = [alloc(st, "a_va%d" % i, [128, 2, 65], BF16) for i in range(3)]
            pb = [alloc(st, "a_pb%d" % i, [128, 2, 4, 128], BF16) for i in range(2)]
            ot = [alloc(st, "a_o%d" % i, [128, D], F32) for i in range(2)]
            den = [alloc(st, "a_den%d" % i, [128, 4], F32) for i in range(2)]
            for i in range(3):
                P.op(POOL, lambda e: e.memset(va[i].t[:], 1.0), writes=[va[i]])

            def prep2(tt):
                s = tt % 2
                r0 = tt * 128
                QKt, QT, KT, VA = qk[s], qT[s], kT[tt % 3], va[tt % 3]
                for b0 in (0, 4, 8):
                    nb = min(4, 9 - b0)
                    bk = bank()
                    for j in range(nb):
                        b = b0 + j
                        P.op(PE, lambda e: e.transpose(out=bk.t[:, j * 128:(j + 1) * 128], in_=QKt.t[:, b * 128:(b + 1) * 128], identity=ident.t[:]),
                             reads=[QKt, ident], writes=[bk])
                    if b0 < 8:
                        src = bk.t[:, 0:512].rearrange("p (k n) -> p k n", k=4)
                        dv = QT.t[:, 2 * b0:2 * b0 + 8, :].rearrange("p (k two) n -> p k two n", two=2)
                        P.op(DVE, lambda e: e.tensor_copy(out=dv[:, :, 0, :], in_=src[0:64]), reads=[bk], writes=[QT])
                        P.op(ACT, lambda e: e.activation(out=dv[:, :, 1, :], in_=src[64:128], func=AF.Copy), reads=[bk], writes=[QT])
                    else:
                        P.op(DVE, lambda e: e.tensor_copy(out=KT.t[:, 0, :], in_=bk.t[0:64, 0:128]), reads=[bk], writes=[KT])
                        P.op(ACT, lambda e: e.activation(out=KT.t[:, 1, :], in_=bk.t[64:128, 0:128], func=AF.Copy), reads=[bk], writes=[KT])
                P.op(POOL, lambda e: e.tensor_copy(out=VA.t[:, :, 0:64], in_=pj[s].t[:, 1152:1280].rearrange("p (g d) -> p g d", g=2)),
                     reads=[pjb[s][2]], writes=[VA])

            def cblocks(tt):
                n = tt % 16
                return ([(kT[(tt - 1) % 3], va[(tt - 1) % 3], mprev)] if n > 0 else []) + [(kT[tt % 3], va[tt % 3], mcur)]

            def scores(tt, g):
                gk, hh = g // 2, g % 2
                h0 = 8 * gk + 4 * hh
                QT = qT[tt % 2]
                PB = pb[(tt * 4 + g) % 2]
                for ci, (KTc, VAc, msk) in enumerate(cblocks(tt)):
                    bk = bank()
                    P.op(PE, lambda e: e.matmul(bk.t[:, :], lhsT=KTc.t[:, gk, :], rhs=QT.t[:, h0:h0 + 4, :].rearrange("p h n -> p (h n)"),
                                                start=True, stop=True), reads=[KTc, QT], writes=[bk])
                    P.op(ACT, lambda e: e.activation(out=PB.t[:, ci, :, :].rearrange("p h n -> p (h n)"), in_=bk.t[:, :], func=AF.Exp, scale=0.125),
                         reads=[bk], writes=[PB])
                    P.op(DVE, lambda e: e.tensor_tensor(out=PB.t[:, ci, :, :], in0=PB.t[:, ci, :, :],
                                                        in1=msk.t[:].unsqueeze(1).broadcast_to([128, 4, 128]), op=ALU.mult),
                         reads=[PB, msk], writes=[PB])

            def pv(tt, g):
                gk, hh = g // 2, g % 2
                h0 = 8 * gk + 4 * hh
                OT = ot[tt % 2]
                PB = pb[(tt * 4 + g) % 2]
                DEN = den[g % 2]
                cbs = cblocks(tt)
                ob = bank()
                for j in range(4):
                    for ci, (KTc, VAc, msk) in enumerate(cbs):
                        P.op(PE, lambda e: e.matmul(ob.t[:, j * 65:(j + 1) * 65], lhsT=PB.t[:, ci, j, :], rhs=VAc.t[:, gk, :],
                                                    start=(ci == 0), stop=(ci == len(cbs) - 1)), reads=[PB, VAc], writes=[ob])
                ov = ob.t[:, 0:260].rearrange("p (h d) -> p h d", h=4)
                P.op(DVE, lambda e: e.tensor_tensor(out=DEN.t[:], in0=ov[:, :, 64], in1=esink.t[:, h0:h0 + 4], op=ALU.add),
                     reads=[ob, esink], writes=[DEN])
                P.op(DVE, lambda e: e.reciprocal(out=DEN.t[:], in_=DEN.t[:]), reads=[DEN], writes=[DEN])
                P.op(DVE, lambda e: e.tensor_tensor(out=OT.t[:, h0 * 64:(h0 + 4) * 64].rearrange("p (h d) -> p h d", h=4), in0=ov[:, :, 0:64],
                                                    in1=DEN.t[:].unsqueeze(2).broadcast_to([128, 4, 64]), op=ALU.mult),
                     reads=[ob, DEN], writes=[OT])


            p1_front(0)
            for tt in range(NT):
                if tt + 1 < NT:
                    p1_front(tt + 1)
                p1_back(tt)
                prep2(tt)
                scores(tt, 0)
                for g in range(4):
                    if g < 3:
                        scores(tt, g + 1)
                    pv(tt, g)
                P.dma(SP, O_d[tt * 128:(tt + 1) * 128, :], ot[tt % 2].t[:], reads=[ot[tt % 2]])
            P.flush()

        if nphase < 2:
            return nc

        if nphase < 3:
            return nc
        with ExitStack() as st:
            A = lambda name, shape, dt=F32: alloc(st, name, shape, dt)
            LR, LI, LDT = A("LR", [128, 32]), A("LI", [128, 32]), A("LDT", [128, 32])
            for tb_, d_ in ((LR, lr_d), (LI, li_d), (LDT, ldt_d)):
                P.dma(SP, tb_.t[:], d_[:, :], writes=[tb_])
            BCAT, BSW = A("BCAT", [128, 32, 16]), A("BSW", [128, 32, 16])
            CA, CB = A("CA", [128, 32, 16]), A("CB", [128, 32, 16])
            for tb_, d_ in ((BCAT, bcat_d), (BSW, bsw_d), (CA, ca_d), (CB, cb_d)):
                P.dma(SP, tb_.t[:].rearrange("p g h -> p (g h)"), d_[:, :], writes=[tb_])
            DSK = A("DSK", [128, 4])
            P.dma(SP, DSK.t[:], dsk_d[:, :], writes=[DSK])
            sgn = A("sgn", [128, 1])
            P.op(POOL, lambda e: e.memset(sgn.t[0:64, :], 1.0), writes=[sgn])
            P.op(POOL, lambda e: e.memset(sgn.t[64:128, :], -1.0), writes=[sgn])
            names = ["dt", "mag", "th", "cs", "sn", "are", "aim", "den", "t1", "t2", "fre", "fim", "tmp", "nfre", "nfims", "fims"]
            S_ = {n_: A("s_" + n_, [128, 32]) for n_ in names}
            tt2 = lambda o, a, b, op, eng=DVE: P.op(eng, lambda e: e.tensor_tensor(out=S_[o].t[:], in0=S_[a].t[:] if isinstance(a, str) else a.t[:],
                                                                                  in1=S_[b].t[:] if isinstance(b, str) else b.t[:], op=op),
                                                    reads=[S_[a] if isinstance(a, str) else a, S_[b] if isinstance(b, str) else b], writes=[S_[o]])
            P.op(ACT, lambda e: e.activation(out=S_["dt"].t[:], in_=LDT.t[:], func=AF.Exp), reads=[LDT], writes=[S_["dt"]])
            tt2("mag", LR, "dt", ALU.mult)
            P.op(ACT, lambda e: e.activation(out=S_["mag"].t[:], in_=S_["mag"].t[:], func=AF.Exp), reads=[S_["mag"]], writes=[S_["mag"]])
            tt2("th", LI, "dt", ALU.mult)
            sin_reduced(st, S_["cs"].t[:], S_["th"].t[:], PI / 2, [S_["th"]], [S_["cs"]], (S_["tmp"].t[:], S_["tmp"]))
            sin_reduced(st, S_["sn"].t[:], S_["th"].t[:], 0.0, [S_["th"]], [S_["sn"]], (S_["tmp"].t[:], S_["tmp"]))
            tt2("are", "mag", "cs", ALU.mult)
            tt2("aim", "mag", "sn", ALU.mult)
            P.op(DVE, lambda e: e.tensor_scalar_add(out=S_["are"].t[:], in0=S_["are"].t[:], scalar1=-1.0), reads=[S_["are"]], writes=[S_["are"]])
            tt2("den", LR, LR, ALU.mult)
            tt2("t1", LI, LI, ALU.mult)
            tt2("den", "den", "t1", ALU.add)
            P.op(DVE, lambda e: e.reciprocal(out=S_["den"].t[:], in_=S_["den"].t[:]), reads=[S_["den"]], writes=[S_["den"]])
            tt2("t1", "are", LR, ALU.mult)
            tt2("t2", "aim", LI, ALU.mult)
            tt2("fre", "t1", "t2", ALU.add)
            tt2("fre", "fre", "den", ALU.mult)
            tt2("t1", "aim", LR, ALU.mult)
            tt2("t2", "are", LI, ALU.mult)
            tt2("fim", "t1", "t2", ALU.subtract)
            tt2("fim", "fim", "den", ALU.mult)
            P.op(DVE, lambda e: e.tensor_scalar(out=S_["nfre"].t[:], in0=S_["fre"].t[:], scalar1=sgn.t[:, 0:1], scalar2=None, op0=ALU.mult),
                 reads=[S_["fre"], sgn], writes=[S_["nfre"]])
            P.op(DVE, lambda e: e.tensor_scalar(out=S_["fims"].t[:], in0=S_["fim"].t[:], scalar1=sgn.t[:, 0:1], scalar2=-1.0, op0=ALU.mult, op1=ALU.mult),
                 reads=[S_["fim"], sgn], writes=[S_["fims"]])
            B1, B2, BT = A("B1", [128, 32, 16]), A("B2", [128, 32, 16]), A("BT", [128, 32, 16])
            bc = lambda n_: S_[n_].t[:].unsqueeze(2).broadcast_to([128, 32, 16])
            P.op(DVE, lambda e: e.tensor_tensor(out=B1.t[:], in0=BCAT.t[:], in1=bc("fre"), op=ALU.mult), reads=[BCAT, S_["fre"]], writes=[B1])
            P.op(DVE, lambda e: e.tensor_tensor(out=BT.t[:], in0=BSW.t[:], in1=bc("fims"), op=ALU.mult), reads=[BSW, S_["fims"]], writes=[BT])
            P.op(DVE, lambda e: e.tensor_tensor(out=B1.t[:], in0=B1.t[:], in1=BT.t[:], op=ALU.add), reads=[B1, BT], writes=[B1])
            P.op(DVE, lambda e: e.tensor_tensor(out=B2.t[:], in0=BSW.t[:], in1=bc("nfre"), op=ALU.mult), reads=[BSW, S_["nfre"]], writes=[B2])
            P.op(DVE, lambda e: e.tensor_tensor(out=BT.t[:], in0=BCAT.t[:], in1=bc("fim"), op=ALU.mult), reads=[BCAT, S_["fim"]], writes=[BT])
            P.op(DVE, lambda e: e.tensor_tensor(out=B2.t[:], in0=B2.t[:], in1=BT.t[:], op=ALU.add), reads=[B2, BT], writes=[B2])
            rmask = A("rmask", [128, 8])
            P.op(POOL, lambda e: e.memset(rmask.t[:], 1.0), writes=[rmask])
            P.op(POOL, lambda e: e.affine_select(out=rmask.t[:], in_=rmask.t[:], pattern=[[-16, 8]], compare_op=ALU.is_ge,
                                                 fill=0.0, base=0, channel_multiplier=1), reads=[rmask], writes=[rmask])
            P.op(POOL, lambda e: e.affine_select(out=rmask.t[:], in_=rmask.t[:], pattern=[[16, 8]], compare_op=ALU.is_ge,
                                                 fill=0.0, base=15, channel_multiplier=-1), reads=[rmask], writes=[rmask])
            B1z, B2z = A("B1z", [128, 32, 128], BF16), A("B2z", [128, 32, 128], BF16)
            C1z, C2z = A("C1z", [128, 32, 128], BF16), A("C2z", [128, 32, 128], BF16)
            for Bsrc, Bz in ((B1, B1z), (B2, B2z)):
                for g4 in range(4):
                    bk = bank()
                    P.op(PE, lambda e, bk=bk, Bsrc=Bsrc, g4=g4: e.transpose(
                        out=bk.t[:, 0:128], in_=Bsrc.t[:, g4 * 8:(g4 + 1) * 8, :].rearrange("p g h -> p (g h)"), identity=ident.t[:]),
                        reads=[Bsrc, ident], writes=[bk])
                    for g8 in range(8):
                        P.op(DVE, lambda e, bk=bk, Bz=Bz, g=g4 * 8 + g8, g8=g8: e.tensor_scalar(
                            out=Bz.t[:, g, :], in0=bk.t[:, 0:128], scalar1=rmask.t[:, g8:g8 + 1], scalar2=None, op0=ALU.mult),
                            reads=[bk, rmask], writes=[Bz])
            P.op(POOL, lambda e: e.memset(C1z.t[:], 0.0), writes=[C1z])
            P.op(POOL, lambda e: e.memset(C2z.t[:], 0.0), writes=[C2z])
            for g in range(32):
                g8 = g % 8
                P.op(DVE, lambda e, g=g, g8=g8: e.tensor_scalar(out=C1z.t[:, g, g8 * 16:(g8 + 1) * 16], in0=CA.t[:, g, :],
                                                                scalar1=sgn.t[:, 0:1], scalar2=None, op0=ALU.mult),
                     reads=[CA, sgn], writes=[C1z])
                P.op(POOL, lambda e, g=g, g8=g8: e.tensor_scalar(out=C2z.t[:, g, g8 * 16:(g8 + 1) * 16], in0=CB.t[:, g, :],
                                                                 scalar1=-1.0, scalar2=None, op0=ALU.mult),
                     reads=[CB], writes=[C2z])
            jj_i = A("jj_i", [128, 128], I32)
            jj = A("jj", [128, 128])
            P.op(POOL, lambda e: e.iota(jj_i.t[:], pattern=[[1, 128]], base=0, channel_multiplier=0), writes=[jj_i])
            P.op(DVE, lambda e: e.tensor_copy(out=jj.t[:], in_=jj_i.t[:]), reads=[jj_i], writes=[jj])
            cosS, sinS, rfull = A("cosS", [128, 32, 128]), A("sinS", [128, 32, 128]), A("rfull", [128, 32, 128])
            with ExitStack() as stt:
                angS = alloc(stt, "angS", [128, 32, 128], F32)
                for g in range(32):
                    P.op(DVE, lambda e, g=g: e.tensor_scalar(out=angS.t[:, g, :], in0=jj.t[:], scalar1=S_["th"].t[:, g:g + 1],
                                                            scalar2=None, op0=ALU.mult), reads=[jj, S_["th"]], writes=[angS])
                    P.op(POOL, lambda e, g=g: e.tensor_copy(out=rfull.t[:, g, :], in_=S_["mag"].t[:, g:g + 1].broadcast_to([128, 128])),
                         reads=[S_["mag"]], writes=[rfull])
                with ExitStack() as stt2:
                    sin_reduced(stt2, cosS.t[:], angS.t[:], PI / 2, [angS], [cosS], None)
                    P.flush()
                with ExitStack() as stt2:
                    sin_reduced(stt2, sinS.t[:], angS.t[:], 0.0, [angS], [sinS], None)
                    P.flush()
            P.op(POOL, lambda e: e.memset(rfull.t[:, :, 0], 0.0), reads=[rfull], writes=[rfull])
            rc = A("rcar", [128, 32])
            thL = A("thL", [128, 32])
            cosL, sinLs = A("cosL", [128, 32]), A("sinLs", [128, 32])
            P.op(DVE, lambda e: e.tensor_scalar_mul(out=thL.t[:], in0=S_["th"].t[:], scalar1=128.0), reads=[S_["th"]], writes=[thL])
            sin_reduced(st, cosL.t[:], thL.t[:], PI / 2, [thL], [cosL], (S_["tmp"].t[:], S_["tmp"]))
            sin_reduced(st, sinLs.t[:], thL.t[:], 0.0, [thL], [sinLs], (S_["tmp"].t[:], S_["tmp"]))
            P.op(DVE, lambda e: e.tensor_scalar(out=sinLs.t[:], in0=sinLs.t[:], scalar1=sgn.t[:, 0:1], scalar2=-1.0, op0=ALU.mult, op1=ALU.mult),
                 reads=[sinLs, sgn], writes=[sinLs])
            permS = A("permS", [128, 128])
            P.op(POOL, lambda e: e.tensor_copy(out=permS.t[:, 0:64], in_=ident.t[:, 64:128]), reads=[ident], writes=[permS])
            P.op(POOL, lambda e: e.tensor_copy(out=permS.t[:, 64:128], in_=ident.t[:, 0:64]), reads=[ident], writes=[permS])

            if dbg3 == 1:
                P.flush()
                return nc
            ut = [A("u_t%d" % i, [128, 512]) for i in range(2)]
            uTb = [A("uTb%d" % i, [128, 4, 128], BF16) for i in range(2)]
            uTf = [A("uTf%d" % i, [128, 4, 128]) for i in range(2)]
            rot = [A("rot%d" % i, [128, 8, 128]) for i in range(2)]
            t1s = A("t1s", [128, 512])
            t2s = A("t2s", [128, 512])
            Gs = [A("Gs%d" % i, [128, 8, 128]) for i in range(2)]
            hc = [A("hc%d" % i, [128, 8, 128], BF16) for i in range(2)]
            hs = [A("hs%d" % i, [128, 8, 128], BF16) for i in range(2)]
            yv = [A("yv%d" % i, [128, 4, 128]) for i in range(2)]
            ge1, ge2 = A("ge1", [128, 512]), A("ge2", [128, 512])
            zt = [A("zt%d" % i, [128, 4, 128], BF16) for i in range(2)]
            last = A("last", [128, 32])
            swp = A("swp", [128, 32])
            carry = A("carry", [128, 32])
            def prep(tt):
                s = tt % 2
                UT, UB, UF = ut[s], uTb[s], uTf[s]
                P.dma(SP, UT.t[:], U_d[tt * 128:(tt + 1) * 128, :], writes=[UT])
                bk = bank()
                for j in range(4):
                    P.op(PE, lambda e: e.transpose(out=bk.t[:, j * 128:(j + 1) * 128], in_=UT.t[:, j * 128:(j + 1) * 128],
                                                   identity=ident.t[:]), reads=[UT, ident], writes=[bk])
                P.op(DVE, lambda e: e.tensor_copy(out=UB.t[:].rearrange("p k n -> p (k n)"), in_=bk.t[:, :]), reads=[bk], writes=[UB])
                P.op(DVE, lambda e: e.tensor_copy(out=UF.t[:].rearrange("p k n -> p (k n)"), in_=bk.t[:, :]), reads=[bk], writes=[UF])

            zfill = A("zfill", [128, D])
            P.op(POOL, lambda e: e.memset(zfill.t[:], 0.0), writes=[zfill])
            prep(0)
            for tt in range(NT):
                s = tt % 2
                n = tt % 16
                r0 = tt * 128
                UT, UB, UF, YV, ZT = ut[s], uTb[s], uTf[s], yv[s], zt[s]
                if tt < NSLOT // 1024:
                    P.dma(SP, XG_d[tt * 1024:(tt + 1) * 1024, :].rearrange("(a p) d -> p a d", p=128),
                          zfill.t[:].unsqueeze(1).broadcast_to([128, 8, D]), reads=[zfill])
                if n == 0:
                    P.op(POOL, lambda e: e.memset(carry.t[:], 0.0), writes=[carry])
                P.op(DVE, lambda e: e.tensor_tensor(out=rc.t[:], in0=carry.t[:], in1=S_["mag"].t[:], op=ALU.mult), reads=[carry, S_["mag"]], writes=[rc])

                def stageA(Q):
                    ROT = rot[Q % 2]
                    for half in range(2):
                        b1, b2 = bank(), bank()
                        for j in range(4):
                            g = Q * 8 + half * 4 + j
                            P.op(PE, lambda e: e.matmul(b1.t[:, j * 128:(j + 1) * 128], lhsT=B1z.t[:, g, :], rhs=UB.t[:, Q, :], start=True, stop=True),
                                 reads=[B1z, UB], writes=[b1])
                        for j in range(4):
                            g = Q * 8 + half * 4 + j
                            P.op(PE, lambda e: e.matmul(b2.t[:, j * 128:(j + 1) * 128], lhsT=B2z.t[:, g, :], rhs=UB.t[:, Q, :], start=True, stop=True),
                                 reads=[B2z, UB], writes=[b2])
                        g0 = Q * 8 + half * 4
                        P.op(DVE, lambda e: e.tensor_tensor(out=t1s.t[:], in0=b1.t[:, :], in1=cosS.t[:, g0:g0 + 4, :].rearrange("p g n -> p (g n)"), op=ALU.mult),
                             reads=[b1, cosS], writes=[t1s])
                        P.op(DVE, lambda e: e.tensor_tensor(out=t2s.t[:], in0=b2.t[:, :], in1=sinS.t[:, g0:g0 + 4, :].rearrange("p g n -> p (g n)"), op=ALU.mult),
                             reads=[b2, sinS], writes=[t2s])
                        P.op(DVE, lambda e: e.tensor_tensor(out=ROT.t[:, half * 4:(half + 1) * 4, :].rearrange("p g n -> p (g n)"), in0=t1s.t[:], in1=t2s.t[:], op=ALU.add),
                             reads=[t1s, t2s], writes=[ROT])

                def stageB(Q):
                    ROT, GS, HC, HS = rot[Q % 2], Gs[Q % 2], hc[Q % 2], hs[Q % 2]
                    P.op(DVE, lambda e: e.tensor_tensor(out=ROT.t[:, :, 0], in0=ROT.t[:, :, 0], in1=rc.t[:, Q * 8:(Q + 1) * 8], op=ALU.add),
                         reads=[ROT, rc], writes=[ROT])
                    P.op(DVE, lambda e: e.tensor_tensor_scan(out=GS.t[:].rearrange("p g n -> p (g n)"),
                                                             data0=rfull.t[:, Q * 8:(Q + 1) * 8, :].rearrange("p g n -> p (g n)"),
                                                             data1=ROT.t[:].rearrange("p g n -> p (g n)"), initial=0.0,
                                                             op0=ALU.mult, op1=ALU.add), reads=[rfull, ROT], writes=[GS])
                    P.op(POOL, lambda e: e.tensor_tensor(out=HS.t[:], in0=GS.t[:], in1=sinS.t[:, Q * 8:(Q + 1) * 8, :], op=ALU.mult),
                         reads=[GS, sinS], writes=[HS])
                    P.op(DVE, lambda e: e.tensor_tensor(out=HC.t[:], in0=GS.t[:], in1=cosS.t[:, Q * 8:(Q + 1) * 8, :], op=ALU.mult),
                         reads=[GS, cosS], writes=[HC])
                    P.op(ACT, lambda e: e.activation(out=last.t[:, Q * 8:(Q + 1) * 8], in_=GS.t[:, :, 127], func=AF.Copy), reads=[GS], writes=[last])

                def stageC(Q):
                    HC, HS = hc[Q % 2], hs[Q % 2]
                    yb = bank()
                    for j in range(8):
                        g = Q * 8 + j
                        P.op(PE, lambda e: e.matmul(yb.t[:, 0:128], lhsT=C1z.t[:, g, :], rhs=HC.t[:, j, :], start=(j == 0), stop=False),
                             reads=[C1z, HC], writes=[yb])
                        P.op(PE, lambda e: e.matmul(yb.t[:, 0:128], lhsT=C2z.t[:, g, :], rhs=HS.t[:, j, :], start=False, stop=(j == 7)),
                             reads=[C2z, HS], writes=[yb])
                    P.op(DVE, lambda e: e.scalar_tensor_tensor(out=YV.t[:, Q, :], in0=UF.t[:, Q, :], scalar=DSK.t[:, Q:Q + 1], in1=yb.t[:, 0:128],
                                                               op0=ALU.mult, op1=ALU.add), reads=[yb, UF, DSK], writes=[YV])

                stageA(0)
                stageA(1)
                if tt + 1 < NT:
                    prep(tt + 1)
                stageB(0)
                stageA(2)
                stageB(1)
                stageC(0)
                stageA(3)
                stageB(2)
                stageC(1)
                stageB(3)
                stageC(2)
                stageC(3)
                P.op(DVE, lambda e: e.tensor_copy(out=swp.t[0:64, :], in_=last.t[64:128, :]), reads=[last], writes=[swp])
                P.op(DVE, lambda e: e.tensor_copy(out=swp.t[64:128, :], in_=last.t[0:64, :]), reads=[last], writes=[swp])
                P.op(DVE, lambda e: e.tensor_tensor(out=swp.t[:], in0=swp.t[:], in1=sinLs.t[:], op=ALU.mult),
                     reads=[swp, sinLs], writes=[swp])
                P.op(DVE, lambda e: e.tensor_tensor(out=carry.t[:], in0=last.t[:], in1=cosL.t[:], op=ALU.mult),
                     reads=[last, cosL], writes=[carry])
                P.op(DVE, lambda e: e.tensor_tensor(out=carry.t[:], in0=carry.t[:], in1=swp.t[:], op=ALU.add),
                     reads=[carry, swp], writes=[carry])
                chk(5)
                yf = YV.t[:].rearrange("p k n -> p (k n)")
                P.op(DVE, lambda e, yf=yf: e.tensor_tensor(out=ge1.t[:], in0=yf, in1=yf, op=ALU.mult), reads=[YV], writes=[ge1])
                P.op(DVE, lambda e: e.tensor_scalar(out=ge1.t[:], in0=ge1.t[:], scalar1=0.044715, scalar2=1.0, op0=ALU.mult, op1=ALU.add),
                     reads=[ge1], writes=[ge1])
                P.op(DVE, lambda e, yf=yf: e.tensor_tensor(out=ge1.t[:], in0=ge1.t[:], in1=yf, op=ALU.mult), reads=[ge1, YV], writes=[ge1])
                P.op(ACT, lambda e: e.activation(out=ge2.t[:], in_=ge1.t[:], func=AF.Tanh, scale=math.sqrt(2.0 / PI)), reads=[ge1], writes=[ge2])
                P.op(DVE, lambda e: e.tensor_scalar(out=ge2.t[:], in0=ge2.t[:], scalar1=0.5, scalar2=0.5, op0=ALU.mult, op1=ALU.add),
                     reads=[ge2], writes=[ge2])
                P.op(DVE, lambda e, yf=yf, ZT=ZT: e.tensor_tensor(out=ZT.t[:].rearrange("p k n -> p (k n)"), in0=ge2.t[:], in1=yf, op=ALU.mult),
                     reads=[ge2, YV], writes=[ZT])
                chk(6)
                P.dma(SP, ZT_d[:, :, r0:r0 + 128], ZT.t[:], reads=[ZT])
                chk(7)
                if debug:
                    P.dma(SP, YDBG_d[:, :, r0:r0 + 128], YV.t[:], reads=[YV])
                chk(8)
                if dbg3 == 3:
                    P.flush()
            P.flush()

        if nphase < 4:
            return nc
        with ExitStack() as st:
            A = lambda name, shape, dt=F32: alloc(st, name, shape, dt)
            lnp = {}
            for k_ in ("ln1_g", "ln1_b", "ln2_g", "ln2_b"):
                lnp[k_] = A(k_, [128, D])
                P.dma(SP, lnp[k_].t[:], ln_d[k_][0, :].partition_broadcast(128), writes=[lnp[k_]])
            brb = A("brb", [128, NE])
            P.dma(SP, brb.t[:], br_d[0, :].partition_broadcast(128), writes=[brb])
            wr = A("wr", [128, 8, NE])
            P.dma(SP, wr.t[:], wr_d.rearrange("(k p) n -> p k n", p=128), writes=[wr])
            triu = A("triu", [128, 128], BF16)
            P.op(POOL, lambda e: e.memset(triu.t[:], 1.0), writes=[triu])
            P.op(POOL, lambda e: e.affine_select(out=triu.t[:], in_=triu.t[:], pattern=[[1, 128]], compare_op=ALU.is_gt,
                                                 fill=0.0, base=0, channel_multiplier=-1), reads=[triu], writes=[triu])
            ones_b = A("ones_b", [128, 128], BF16)
            P.op(POOL, lambda e: e.memset(ones_b.t[:], 1.0), writes=[ones_b])
            ecap = A("ecap", [128, NE])
            ecap_i = A("ecap_i", [128, NE], I32)
            P.op(POOL, lambda e: e.iota(ecap_i.t[:], pattern=[[CAP, NE]], base=0, channel_multiplier=0), writes=[ecap_i])
            P.op(DVE, lambda e: e.tensor_copy(out=ecap.t[:], in_=ecap_i.t[:]), reads=[ecap_i], writes=[ecap])
            junk_i = A("junk_i", [128, 1], I32)
            junk = A("junk", [128, 1])
            P.op(POOL, lambda e: e.iota(junk_i.t[:], pattern=[[0, 1]], base=NSLOT, channel_multiplier=1), writes=[junk_i])
            P.op(DVE, lambda e: e.tensor_copy(out=junk.t[:], in_=junk_i.t[:]), reads=[junk_i], writes=[junk])
            cnt_run = A("cnt_run", [128, NE])
            P.op(POOL, lambda e: e.memset(cnt_run.t[:], 0.0), writes=[cnt_run])
            KcT = [A("KcT%d" % i, [128, 8, 256], BF16) for i in range(NSEQ)]
            Vc = [A("Vc%d" % i, [128, 2, D], BF16) for i in range(NSEQ)]
            with ExitStack() as st2:
                stage = [alloc(st2, "stg4_%d" % i, [128, 1024], F32) for i in range(2)]
                P.op(POOL, lambda e: e.memset(stage[0].t[:], 0.0), writes=[stage[0]])
                P.dma(SP, YG_d[NSLOT:NSLOT + 128, :], stage[0].t[:], reads=[stage[0]])
                Wk = load_weight_bf16(st2, "Wk", wk_d, D, D, stage)
                Wv = load_weight_bf16(st2, "Wv", wv_d, D, D, stage)
                memt = [alloc(st2, "memt%d" % i, [128, D], F32) for i in range(2)]
                memT = alloc(st2, "memT", [128, 8, 256], BF16)
                for sq in range(NSEQ):
                    for mt in range(2):
                        M_ = memt[mt]
                        P.dma(SP, M_.t[:], mem_d[sq * 256 + mt * 128: sq * 256 + (mt + 1) * 128, :], writes=[M_])
                        transpose_tile(memT, lambda b0, nb, mt=mt: memT.t[:, b0:b0 + nb, mt * 128:(mt + 1) * 128], M_,
                                       lambda b, M_=M_: M_.t[:, b * 128:(b + 1) * 128], 8)
                    for j in range(8):
                        bk = bank()
                        for k in range(8):
                            P.op(PE, lambda e, bk=bk, k=k, j=j: e.matmul(bk.t[:, 0:256], lhsT=Wk.t[:, k, j * 128:(j + 1) * 128],
                                                                         rhs=memT.t[:, k, :], start=(k == 0), stop=(k == 7)),
                                 reads=[Wk, memT], writes=[bk])
                        evac(KcT[sq].t[:, j, :], bk.t[:, 0:256], [bk], [KcT[sq]])
                    for mt in range(2):
                        for hf in range(2):
                            bk = bank()
                            for k in range(8):
                                P.op(PE, lambda e, bk=bk, k=k, mt=mt, hf=hf: e.matmul(
                                    bk.t[:, :], lhsT=memT.t[:, k, mt * 128:(mt + 1) * 128], rhs=Wv.t[:, k, hf * 512:(hf + 1) * 512],
                                    start=(k == 0), stop=(k == 7)), reads=[Wv, memT], writes=[bk])
                            evac(Vc[sq].t[:, mt, hf * 512:(hf + 1) * 512], bk.t[:, :], [bk], [Vc[sq]])
                P.flush()
            Wo, Wa, Wb = A("Wo", [128, 8, D], BF16), A("Wa", [128, 4, D], BF16), A("Wb", [128, 4, D], BF16)
            Wout, Wq, Woc = A("Wout", [128, 8, D], BF16), A("Wq", [128, 8, D], BF16), A("Woc", [128, 8, D], BF16)
            with ExitStack() as stw:
                stage_w = [alloc(stw, "stgw%d" % i, [128, 1024], F32) for i in range(2)]
                for W_, d_, K_ in ((Wo, w_attn_o_d, D), (Wa, w_glu_a_d, 512), (Wb, w_glu_b_d, 512), (Wout, w_out_d, D), (Wq, wq_d, D), (Woc, wo_d, D)):
                    fill_weight(W_, d_, K_, D, stage_w)
                P.flush()
            lanes = []
            for L in range(3):
                n_ = lambda x: "p4_%s%d" % (x, L)
                lanes.append(dict(
                    Z=A(n_("z"), [128, 4, 128], BF16), G=A(n_("g"), [128, 2048], BF16),
                    aT=A(n_("aT"), [128, 8, 128], BF16), tb=A(n_("tb"), [128, D]), m1=A(n_("m1"), [128, D]), xin=A(n_("xin"), [128, D]),
                    qcT=A(n_("qcT"), [128, 8, 128], BF16), pe=A(n_("pe"), [128, 4, 256]), pT=A(n_("pT"), [128, 8, 128], BF16),
                    stats=A(n_("stats"), [128, 2, 6]), mv=A(n_("mv"), [128, 2]), rstd=A(n_("rstd"), [128, 1]),
                    mx=A(n_("mx"), [128, 4]), nmx=A(n_("nmx"), [128, 4]), ssum=A(n_("ssum"), [128, 4])))
            x2T = A("p4_x2T", [128, 8, 128])
            lg = A("p4_lg", [128, NE])
            top8 = A("p4_top8", [128, 8])
            msk = A("p4_msk", [128, NE])
            mskb = A("p4_mskb", [128, NE], BF16)
            ex = A("p4_ex", [128, NE])
            nv0 = A("p4_nv0", [128, 1])
            esum = A("p4_esum", [128, 1])
            gate = A("p4_gate", [128, NE])
            dest = A("p4_dest", [128, NE])
            oh = A("p4_oh", [128, NE])
            ohd = A("p4_ohd", [128, NE])
            dkf = A("p4_dkf", [128, 4])

            def linear2(lhsT_tb, nk, W):
                bks = [bank(), bank()]
                for hf in range(2):
                    for k in range(nk):
                        P.op(PE, lambda e: e.matmul(bks[hf].t[:, :], lhsT=lhsT_tb.t[:, k, :], rhs=W.t[:, k, hf * 512:(hf + 1) * 512],
                                                    start=(k == 0), stop=(k == nk - 1)), reads=[lhsT_tb, W], writes=[bks[hf]])
                return bks

            def make_tile(tt):
                d = lanes[tt % 3]
                sq = tt // 16
                r0 = tt * 128
                Z_, G_, aT, tb, m1, xin = d["Z"], d["G"], d["aT"], d["tb"], d["m1"], d["xin"]
                qcT, pe_, pT, X2 = d["qcT"], d["pe"], d["pT"], d["xin"]
                O_ap = pe_.t[:].rearrange("p h m -> p (h m)")
                lnt = (d["stats"], d["mv"], d["rstd"])
                mx, nmx, ssum = d["mx"], d["nmx"], d["ssum"]
                S = {}
                HS = [slice(0, 512), slice(512, 1024)]

                def to_T(src):
                    transpose_tile(aT, lambda b0, nb: aT.t[:, b0:b0 + nb, :], src, lambda b: src.t[:, b * 128:(b + 1) * 128], 8)

                def s0():
                    P.dma(SP, O_ap, O_d[r0:r0 + 128, :], writes=[pe_])
                    P.dma(SP, Z_.t[:], ZT_d[:, :, r0:r0 + 128], writes=[Z_])
                    P.dma(SP, G_.t[:], G_d[r0:r0 + 128, :], writes=[G_])
                    P.dma(SP, xin.t[:], x_d[r0:r0 + 128, :], writes=[xin])
                    transpose_tile(aT, lambda b0, nb: aT.t[:, b0:b0 + nb, :], pe_, lambda b: O_ap[:, b * 128:(b + 1) * 128], 8)

                def s1():
                    S["ba"] = linear2(aT, 8, Wo)

                def s1b():
                    ba = S["ba"]
                    for hf in range(2):
                        sl = HS[hf]
                        P.op(DVE, lambda e: e.scalar_tensor_tensor(out=m1.t[:, sl], in0=G_.t[:, sl], scalar=1.0, in1=ba[hf].t[:, :],
                                                                   op0=ALU.add, op1=ALU.mult), reads=[G_, ba[hf]], writes=[m1])
                    rel(*ba)
                    S["bgb"] = linear2(Z_, 4, Wb)

                def s2():
                    bgb = S["bgb"]
                    for hf in range(2):
                        sl = HS[hf]
                        P.op(ACT, lambda e: e.activation(out=tb.t[:, sl], in_=bgb[hf].t[:, :], func=AF.Tanh, scale=0.5), reads=[bgb[hf]], writes=[tb])
                    rel(*bgb)
                    S["bga"] = linear2(Z_, 4, Wa)

                def s3():
                    bga = S["bga"]
                    for hf in range(2):
                        sl = HS[hf]
                        P.op(DVE, lambda e: e.scalar_tensor_tensor(out=tb.t[:, sl], in0=tb.t[:, sl], scalar=1.0, in1=bga[hf].t[:, :],
                                                                   op0=ALU.add, op1=ALU.mult), reads=[tb, bga[hf]], writes=[tb])
                    rel(*bga)
                    P.op(DVE, lambda e: e.scalar_tensor_tensor(out=tb.t[:], in0=G_.t[:, 1024:2048], scalar=1.0, in1=tb.t[:],
                                                               op0=ALU.add, op1=ALU.mult), reads=[G_, tb], writes=[tb])
                    P.op(DVE, lambda e: e.scalar_tensor_tensor(out=m1.t[:], in0=tb.t[:], scalar=0.5, in1=m1.t[:],
                                                               op0=ALU.mult, op1=ALU.add), reads=[tb, m1], writes=[m1])
                    to_T(m1)

                def s4():
                    S["bm"] = linear2(aT, 8, Wout)
                    P.op(ACT, lambda e: e.activation(out=xin.t[:], in_=xin.t[:], func=AF.Copy, scale=ALPHA), reads=[xin], writes=[xin])

                def s5():
                    bm = S["bm"]
                    for hf in range(2):
                        sl = HS[hf]
                        P.op(DVE, lambda e: e.scalar_tensor_tensor(out=xin.t[:, sl], in0=bm[hf].t[:, :], scalar=0.5, in1=xin.t[:, sl],
                                                                   op0=ALU.mult, op1=ALU.add), reads=[bm[hf], xin], writes=[xin])
                    rel(*bm)
                    layer_norm(lnt, xin, lnp["ln1_g"], lnp["ln1_b"], tb)
                    to_T(tb)

                def s6():
                    for j2 in range(2):
                        bq = bank()
                        for j in range(4 * j2, 4 * j2 + 4):
                            for k in range(8):
                                P.op(PE, lambda e: e.matmul(bq.t[:, (j % 4) * 128:(j % 4 + 1) * 128], lhsT=Wq.t[:, k, j * 128:(j + 1) * 128],
                                                            rhs=aT.t[:, k, :], start=(k == 0), stop=(k == 7)), reads=[Wq, aT], writes=[bq])
                        evac(qcT.t[:, 4 * j2:4 * j2 + 4, :].rearrange("p k n -> p (k n)"), bq.t[:, :], [bq], [qcT])
                        rel(bq)
                    bs = [bank(), bank()]
                    for hh in range(4):
                        for dc in range(2):
                            P.op(PE, lambda e: e.matmul(bs[hh // 2].t[:, (hh % 2) * 256:(hh % 2 + 1) * 256], lhsT=qcT.t[:, 2 * hh + dc, :],
                                                        rhs=KcT[sq].t[:, 2 * hh + dc, :], start=(dc == 0), stop=(dc == 1)),
                                 reads=[qcT, KcT[sq]], writes=[bs[hh // 2]])
                    S["bs"] = bs

                def s7():
                    bs = S["bs"]
                    for b2_ in range(2):
                        P.op(DVE, lambda e: e.reduce_max(out=mx.t[:, 2 * b2_:2 * b2_ + 2], in_=bs[b2_].t[:, :].rearrange("p (h m) -> p h m", h=2),
                                                         axis=AX.X), reads=[bs[b2_]], writes=[mx])
                    P.op(DVE, lambda e: e.tensor_scalar_mul(out=nmx.t[:], in0=mx.t[:], scalar1=-1.0 / 16.0), reads=[mx], writes=[nmx])
                    for hh in range(4):
                        P.op(ACT, lambda e: e.activation(out=pe_.t[:, hh, :], in_=bs[hh // 2].t[:, (hh % 2) * 256:(hh % 2 + 1) * 256], func=AF.Exp,
                                                         bias=nmx.t[:, hh:hh + 1], scale=1.0 / 16.0, accum_out=ssum.t[:, hh:hh + 1]),
                             reads=[bs[hh // 2], nmx], writes=[pe_, ssum])
                    rel(*bs)
                    P.op(DVE, lambda e: e.reciprocal(out=ssum.t[:], in_=ssum.t[:]), reads=[ssum], writes=[ssum])
                    transpose_tile(pT, lambda b0, nb: pT.t[:, b0:b0 + nb, :], pe_,
                                   lambda b: pe_.t[:, b // 2, (b % 2) * 128:(b % 2 + 1) * 128], 8)

                def s8():
                    bo = [bank(), bank()]
                    for hh in range(4):
                        for mc in range(2):
                            P.op(PE, lambda e: e.matmul(bo[hh // 2].t[:, (hh % 2) * 256:(hh % 2 + 1) * 256], lhsT=pT.t[:, 2 * hh + mc, :],
                                                        rhs=Vc[sq].t[:, mc, hh * 256:(hh + 1) * 256], start=(mc == 0), stop=(mc == 1)),
                                 reads=[pT, Vc[sq]], writes=[bo[hh // 2]])
                    for hh in range(4):
                        P.op(DVE, lambda e: e.tensor_scalar(out=m1.t[:, hh * 256:(hh + 1) * 256], in0=bo[hh // 2].t[:, (hh % 2) * 256:(hh % 2 + 1) * 256],
                                                            scalar1=ssum.t[:, hh:hh + 1], scalar2=None, op0=ALU.mult),
                             reads=[bo[hh // 2], ssum], writes=[m1])
                    rel(*bo)
                    to_T(m1)

                def s9():
                    bc_ = linear2(aT, 8, Woc)
                    P.op(ACT, lambda e: e.activation(out=xin.t[:], in_=tb.t[:], func=AF.Copy, scale=ALPHA), reads=[tb], writes=[xin])
                    for hf in range(2):
                        sl = HS[hf]
                        P.op(DVE, lambda e: e.tensor_tensor(out=xin.t[:, sl], in0=bc_[hf].t[:, :], in1=xin.t[:, sl], op=ALU.add),
                             reads=[bc_[hf], xin], writes=[xin])
                    rel(*bc_)
                    layer_norm(lnt, xin, lnp["ln2_g"], lnp["ln2_b"], X2)
                    P.dma(SP, X2_d[r0:r0 + 128, :], X2.t[:], reads=[X2])

                def s10():
                    transpose_tile(x2T, lambda b0, nb: x2T.t[:, b0:b0 + nb, :], X2, lambda b: X2.t[:, b * 128:(b + 1) * 128], 8)
                    bl_ = bank()
                    for k in range(8):
                        P.op(PE, lambda e: e.matmul(bl_.t[:, 0:NE], lhsT=x2T.t[:, k, :], rhs=wr.t[:, k, :], start=(k == 0), stop=(k == 7)),
                             reads=[x2T, wr], writes=[bl_])
                    P.op(DVE, lambda e: e.tensor_tensor(out=lg.t[:], in0=bl_.t[:, 0:NE], in1=brb.t[:], op=ALU.add), reads=[bl_, brb], writes=[lg])
                    rel(bl_)
                    P.op(DVE, lambda e: e.max(out=top8.t[:], in_=lg.t[:]), reads=[lg], writes=[top8])
                    P.op(DVE, lambda e: e.tensor_scalar(out=msk.t[:], in0=lg.t[:], scalar1=top8.t[:, 3:4], scalar2=None, op0=ALU.is_ge),
                         reads=[lg, top8], writes=[msk])
                    P.op(DVE, lambda e: e.tensor_copy(out=mskb.t[:], in_=msk.t[:]), reads=[msk], writes=[mskb])
                    P.op(DVE, lambda e: e.tensor_scalar_mul(out=nv0.t[:], in0=top8.t[:, 0:1], scalar1=-1.0), reads=[top8], writes=[nv0])
                    P.op(ACT, lambda e: e.activation(out=ex.t[:], in_=lg.t[:], func=AF.Exp, bias=nv0.t[:, 0:1], scale=1.0), reads=[lg, nv0], writes=[ex])
                    P.op(DVE, lambda e: e.tensor_tensor(out=ex.t[:], in0=ex.t[:], in1=msk.t[:], op=ALU.mult), reads=[ex, msk], writes=[ex])
                    P.op(DVE, lambda e: e.reduce_sum(out=esum.t[:], in_=ex.t[:], axis=AX.X), reads=[ex], writes=[esum])
                    P.op(DVE, lambda e: e.reciprocal(out=esum.t[:], in_=esum.t[:]), reads=[esum], writes=[esum])
                    P.op(DVE, lambda e: e.tensor_scalar(out=gate.t[:], in0=ex.t[:], scalar1=esum.t[:, 0:1], scalar2=None, op0=ALU.mult),
                         reads=[ex, esum], writes=[gate])
                    brk = bank()
                    P.op(PE, lambda e: e.matmul(brk.t[:, 0:NE], lhsT=triu.t[:], rhs=mskb.t[:], start=True, stop=True), reads=[triu, mskb], writes=[brk])
                    P.op(PE, lambda e: e.matmul(brk.t[:, 64:64 + NE], lhsT=ones_b.t[:], rhs=mskb.t[:], start=True, stop=True),
                         reads=[ones_b, mskb], writes=[brk])
                    P.op(DVE, lambda e: e.tensor_tensor(out=dest.t[:], in0=brk.t[:, 0:NE], in1=cnt_run.t[:], op=ALU.add), reads=[brk, cnt_run], writes=[dest])
                    P.op(DVE, lambda e: e.tensor_tensor(out=cnt_run.t[:], in0=brk.t[:, 64:64 + NE], in1=cnt_run.t[:], op=ALU.add),
                         reads=[brk, cnt_run], writes=[cnt_run])
                    rel(brk)
                    P.op(DVE, lambda e: e.tensor_scalar(out=oh.t[:], in0=dest.t[:], scalar1=float(CAP) - 0.5, scalar2=None, op0=ALU.is_lt),
                         reads=[dest], writes=[oh])
                    P.op(DVE, lambda e: e.tensor_tensor(out=gate.t[:], in0=gate.t[:], in1=oh.t[:], op=ALU.mult), reads=[gate, oh], writes=[gate])
                    P.op(DVE, lambda e: e.tensor_tensor(out=dest.t[:], in0=dest.t[:], in1=ecap.t[:], op=ALU.add), reads=[dest, ecap], writes=[dest])
                    P.op(DVE, lambda e: e.tensor_scalar(out=dest.t[:], in0=dest.t[:], scalar1=junk.t[:, 0:1], scalar2=None, op0=ALU.subtract),
                         reads=[dest, junk], writes=[dest])
                    P.op(DVE, lambda e: e.tensor_tensor(out=dest.t[:], in0=dest.t[:], in1=oh.t[:], op=ALU.mult), reads=[dest, oh], writes=[dest])
                    P.op(DVE, lambda e: e.tensor_scalar(out=dest.t[:], in0=dest.t[:], scalar1=junk.t[:, 0:1], scalar2=None, op0=ALU.add),
                         reads=[dest, junk], writes=[dest])
                    for k in range(4):
                        P.op(DVE, lambda e: e.tensor_scalar(out=oh.t[:], in0=lg.t[:], scalar1=top8.t[:, k:k + 1], scalar2=None, op0=ALU.is_equal),
                             reads=[lg, top8], writes=[oh])
                        P.op(DVE, lambda e: e.tensor_tensor(out=ohd.t[:], in0=oh.t[:], in1=dest.t[:], op=ALU.mult), reads=[oh, dest], writes=[ohd])
                        P.op(DVE, lambda e: e.reduce_sum(out=dkf.t[:, k:k + 1], in_=ohd.t[:], axis=AX.X), reads=[ohd], writes=[dkf])
                        P.op(DVE, lambda e: e.tensor_tensor(out=ohd.t[:], in0=oh.t[:], in1=gate.t[:], op=ALU.mult), reads=[oh, gate], writes=[ohd])
                        P.op(DVE, lambda e: e.reduce_sum(out=GK.t[:, tt, k:k + 1], in_=ohd.t[:], axis=AX.X), reads=[ohd], writes=[GK])
                    P.op(DVE, lambda e: e.tensor_copy(out=DK.t[:, tt, :], in_=dkf.t[:]), reads=[dkf], writes=[DK])
                    for k in range(4):
                        P.op(POOL, lambda e: e.indirect_dma_start(
                            out=XG_d[:, :], out_offset=bass.IndirectOffsetOnAxis(ap=DK.t[:, tt, k:k + 1], axis=0),
                            in_=X2.t[:], in_offset=None), reads=[X2, DK], dma=True)

                return [s0, s1, s1b, s2, s3, s4, s5, s6, s7, s8, s9, s10]

            strict[0] = True
            for t0_ in range(0, NT, 3):
                grp_ = [make_tile(t_) for t_ in range(t0_, min(NT, t0_ + 3))]
                for k_ in range(len(grp_[0])):
                    for tl_ in grp_:
                        tl_[k_]()
            strict[0] = False
            for i_ in range(8):
                live[i_] = False
            P.dma(SP, cnt_d[:, :], cnt_run.t[:], reads=[cnt_run])
            P.flush()

        if nphase < 5:
            return nc
        with ExitStack() as st:
            A = lambda name, shape, dt=F32: alloc(st, name, shape, dt)
            NBG, NBL = A("NBG", [128, NE, 8]), A("NBL", [128, NE, 8])
            P.dma(SP, NBG.t[:].rearrange("p e c -> p (e c)"), bg_d[:, :], writes=[NBG])
            P.dma(SP, NBL.t[:].rearrange("p e c -> p (e c)"), bl_d[:, :], writes=[NBL])
            for T_ in (NBG, NBL):
                P.op(DVE, lambda e, T_=T_: e.tensor_scalar(out=T_.t[:], in0=T_.t[:], scalar1=-1.0, scalar2=7.0, op0=ALU.mult, op1=ALU.add),
                     reads=[T_], writes=[T_])
            stg = [A("stg5_%d" % i, [128, 2048]) for i in range(3)]
            Wg = [A("Wg%d" % i, [128, 8, 2, 1024], BF16) for i in range(2)]
            Wd = [A("Wd%d" % i, [128, 8, 1024], BF16) for i in range(2)]
            bdb = [A("bdb%d" % i, [128, D]) for i in range(2)]
            xg = [A("xg%d" % i, [128, D]) for i in range(2)]
            XT = [A("XgT%d" % i, [128, 8, CAP], BF16) for i in range(2)]
            actT = A("actT", [128, 8, CAP], BF16)
            rg = [A("rg%d" % i, [128, 512]) for i in range(2)]
            sv = [A("sv%d" % i, [128, 512]) for i in range(2)]
            rl = [A("rl%d" % i, [128, 512]) for i in range(2)]
            yt = [A("yt%d" % i, [128, D]) for i in range(2)]
            stg_rr = [0]

            def weight_chunks(ex_):
                s_ = ex_ % 2
                WG, WD, BD = Wg[s_], Wd[s_], bdb[s_]
                th = []
                for k in range(8):
                    def f(k=k):
                        sg = stg[stg_rr[0] % 3]
                        stg_rr[0] += 1
                        P.dma(SP, sg.t[:], wgu_d[ex_, k * 128:(k + 1) * 128, :], writes=[sg])
                        evac(WG.t[:, k, :, :], sg.t[:].rearrange("p (f two) -> p two f", two=2), [sg], [WG], engs=(ACT, DVE))
                    th.append(f)
                for k2 in range(4):
                    def f(k2=k2):
                        sg = stg[stg_rr[0] % 3]
                        stg_rr[0] += 1
                        P.dma(SP, sg.t[:].rearrange("p (a n) -> p a n", a=2),
                              wd_d[ex_, k2 * 256:(k2 + 1) * 256, :].rearrange("(a p) n -> p a n", p=128), writes=[sg])
                        evac(WD.t[:, 2 * k2:2 * k2 + 2, :], sg.t[:].rearrange("p (a n) -> p a n", a=2), [sg], [WD], engs=(ACT, DVE))
                    th.append(f)
                th.append(lambda: P.dma(SP, BD.t[:], bd_d[ex_, :].partition_broadcast(128), writes=[BD]))
                return th

            def prep_x(ex_):
                XGT = XT[ex_ % 2]
                for stl in range(CAP // 128):
                    XGt = xg[stl % 2]
                    P.dma(SP, XGt.t[:], XG_d[ex_ * CAP + stl * 128: ex_ * CAP + (stl + 1) * 128, :], writes=[XGt])
                    transpose_tile(XGT, lambda b0, nb, stl=stl: XGT.t[:, b0:b0 + nb, stl * 128:(stl + 1) * 128], XGt,
                                   lambda b, XGt=XGt: XGt.t[:, b * 128:(b + 1) * 128], 8)

            for f in weight_chunks(0):
                f()
            prep_x(0)
            for ex_ in range(NE):
                s_ = ex_ % 2
                WG, WD, BD, XGT = Wg[s_], Wd[s_], bdb[s_], XT[s_]
                pend = weight_chunks(ex_ + 1) if ex_ + 1 < NE else []
                for (c0, cw) in ((0, 512), (512, CAP - 512)):
                    for fc in range(8):
                        i2 = fc % 2
                        bg_, bl2 = bank(), bank()
                        for k in range(8):
                            P.op(PE, lambda e, k=k: e.matmul(bg_.t[:, 0:cw], lhsT=WG.t[:, k, 0, fc * 128:(fc + 1) * 128], rhs=XGT.t[:, k, c0:c0 + cw],
                                                            start=(k == 0), stop=(k == 7)), reads=[WG, XGT], writes=[bg_])
                        for k in range(8):
                            P.op(PE, lambda e, k=k: e.matmul(bl2.t[:, 0:cw], lhsT=WG.t[:, k, 1, fc * 128:(fc + 1) * 128], rhs=XGT.t[:, k, c0:c0 + cw],
                                                            start=(k == 0), stop=(k == 7)), reads=[WG, XGT], writes=[bl2])
                        RG, SV, RL = rg[i2], sv[i2], rl[i2]
                        P.op(ACT, lambda e: e.activation(out=RG.t[:, 0:cw], in_=bg_.t[:, 0:cw], func=AF.Relu, bias=NBG.t[:, ex_, fc:fc + 1], scale=-1.0),
                             reads=[bg_, NBG], writes=[RG])
                        P.op(ACT, lambda e: e.activation(out=SV.t[:, 0:cw], in_=RG.t[:, 0:cw], func=AF.Silu, bias=nb7.t[:, 0:1], scale=-1.702),
                             reads=[RG, nb7], writes=[SV])
                        P.op(ACT, lambda e: e.activation(out=RL.t[:, 0:cw], in_=bl2.t[:, 0:cw], func=AF.Relu, bias=NBL.t[:, ex_, fc:fc + 1], scale=-1.0),
                             reads=[bl2, NBL], writes=[RL])
                        P.op(DVE, lambda e: e.tensor_scalar(out=RL.t[:, 0:cw], in0=RL.t[:, 0:cw], scalar1=14.0, scalar2=-8.0, op0=ALU.min, op1=ALU.add),
                             reads=[RL], writes=[RL])
                        P.op(DVE, lambda e: e.scalar_tensor_tensor(out=actT.t[:, fc, c0:c0 + cw], in0=RL.t[:, 0:cw], scalar=-1.0, in1=SV.t[:, 0:cw],
                                                                   op0=ALU.mult, op1=ALU.mult), reads=[RL, SV], writes=[actT])
                        if pend:
                            pend.pop(0)()
                if ex_ + 1 < NE:
                    prep_x(ex_ + 1)
                for stl in range(CAP // 128):
                    YT = yt[stl % 2]
                    for hf in range(2):
                        bk = bank()
                        for fc in range(8):
                            P.op(PE, lambda e, fc=fc: e.matmul(bk.t[:, :], lhsT=actT.t[:, fc, stl * 128:(stl + 1) * 128], rhs=WD.t[:, fc, hf * 512:(hf + 1) * 512],
                                                              start=(fc == 0), stop=(fc == 7)), reads=[actT, WD], writes=[bk])
                        P.op(DVE, lambda e: e.scalar_tensor_tensor(out=YT.t[:, hf * 512:(hf + 1) * 512], in0=bk.t[:, :], scalar=1.0 / 1.702,
                                                                   in1=BD.t[:, hf * 512:(hf + 1) * 512], op0=ALU.mult, op1=ALU.add),
                             reads=[bk, BD], writes=[YT])
                    P.dma(POOL, YG_d[ex_ * CAP + stl * 128: ex_ * CAP + (stl + 1) * 128, :], YT.t[:], reads=[YT])
                    if pend:
                        pend.pop(0)()
                while pend:
                    pend.pop(0)()
            P.flush()

        if nphase < 6:
            return nc
        with ExitStack() as st:
            A = lambda name, shape, dt=F32: alloc(st, name, shape, dt)
            g3, b3 = A("g3", [128, D]), A("b3", [128, D])
            P.dma(SP, g3.t[:], ln_d["ln3_g"][0, :].partition_broadcast(128), writes=[g3])
            P.dma(SP, b3.t[:], ln_d["ln3_b"][0, :].partition_broadcast(128), writes=[b3])
            x2t = [A("c_x2_%d" % i, [128, D]) for i in range(2)]
            yk = [[A("c_y%d_%d" % (i, k), [128, D]) for k in range(4)] for i in range(2)]
            acc = [A("c_acc%d" % i, [128, D]) for i in range(2)]
            res = [A("c_res%d" % i, [128, D]) for i in range(2)]
            stats, mv, rstd = A("c_stats", [128, 2, 6]), A("c_mv", [128, 2]), A("c_rstd", [128, 1])
            for tt in range(NT):
                s = tt % 2
                r0 = tt * 128
                X2, ACC, RES = x2t[s], acc[s], res[s]
                P.dma(SP, X2.t[:], X2_d[r0:r0 + 128, :], writes=[X2])
                for k in range(4):
                    P.op(POOL, lambda e, k=k, tt=tt, s=s: e.indirect_dma_start(
                        out=yk[s][k].t[:], out_offset=None, in_=YG_d[:, :],
                        in_offset=bass.IndirectOffsetOnAxis(ap=DK.t[:, tt, k:k + 1], axis=0)), reads=[DK], writes=[yk[s][k]], dma=True)
                P.op(ACT, lambda e, X2=X2, ACC=ACC: e.activation(out=ACC.t[:], in_=X2.t[:], func=AF.Copy, scale=ALPHA), reads=[X2], writes=[ACC])
                for k in range(4):
                    P.op(DVE, lambda e, k=k, tt=tt, s=s, ACC=ACC: e.scalar_tensor_tensor(
                        out=ACC.t[:], in0=yk[s][k].t[:], scalar=GK.t[:, tt, k:k + 1], in1=ACC.t[:], op0=ALU.mult, op1=ALU.add),
                        reads=[yk[s][k], GK, ACC], writes=[ACC])
                layer_norm((stats, mv, rstd), ACC, g3, b3, RES)
                P.dma(SP, out_d[r0:r0 + 128, :], RES.t[:], reads=[RES])
            P.flush()
    except _Stop:
        pass
    return nc


def _core_inputs(inp, c):
    f = lambda a: np.ascontiguousarray(a, dtype=np.float32)
    b0 = c * NSEQ
    m = {}
    m["x"] = f(inp["x"][b0:b0 + NSEQ].reshape(TOK, D))
    m["mem"] = f(inp["mem"][b0:b0 + NSEQ].reshape(NSEQ * 256, D))
    pos = np.asarray(inp["positions"][b0:b0 + NSEQ]).reshape(NT, 128).astype(np.int32)
    m["pos_t"] = np.ascontiguousarray(pos.T)
    return m


def _shared_inputs(inp):
    f = lambda a: np.ascontiguousarray(a, dtype=np.float32)
    m = {}
    for k in ("w_in", "w_attn_o", "w_glu_a", "w_glu_b", "w_out", "wq_c", "wk_c", "wv_c", "wo_c", "w_router",
              "w_gate_up", "w_down", "b_down"):
        m[k] = f(np.asarray(inp[k])[0])
    for k in ("sinks", "ln1_g", "ln1_b", "ln2_g", "ln2_b", "ln3_g", "ln3_b", "b_router"):
        m[k] = f(np.asarray(inp[k]).reshape(1, -1))
    lr = np.asarray(inp["lam_re"])[0]
    li = np.asarray(inp["lam_im"])[0]
    m["lamre_t"] = f(np.concatenate([lr.T, lr.T], 0))
    m["lamim_t"] = f(np.concatenate([li.T, li.T], 0))
    m["logdt_t"] = f(np.broadcast_to(np.asarray(inp["log_dt"])[0][None, :], (128, 32)))
    bre = np.asarray(inp["b_re"])[0].transpose(1, 0, 2)
    bim = np.asarray(inp["b_im"])[0].transpose(1, 0, 2)
    m["bcat"] = f(np.concatenate([bre, bim], 0).reshape(128, 512))
    m["bsw"] = f(np.concatenate([bim, bre], 0).reshape(128, 512))
    cre = np.asarray(inp["c_re"])[0].transpose(2, 0, 1)
    cim = np.asarray(inp["c_im"])[0].transpose(2, 0, 1)
    m["ca"] = f(np.concatenate([cre, cim], 0).reshape(128, 512))
    m["cb"] = f(np.concatenate([cim, cre], 0).reshape(128, 512))
    m["dskip_t"] = f(np.asarray(inp["d_skip"])[0].reshape(4, 128).T)
    bgu = np.asarray(inp["b_gate_up"])[0]
    bg = bgu[:, 0::2].reshape(NE, 8, 128).transpose(2, 0, 1)
    bl = bgu[:, 1::2].reshape(NE, 8, 128).transpose(2, 0, 1)
    m["bg_t"] = f(bg.reshape(128, NE * 8))
    m["bl_t"] = f(bl.reshape(128, NE * 8))
    return m


_NC_CACHE = {}


def kernel(**inputs):
    if "nc" not in _NC_CACHE:
        _NC_CACHE["nc"] = build_nc()
    nc = _NC_CACHE["nc"]
    shared = _shared_inputs(inputs)
    in_maps = []
    for c in range(NCORES):
        m = dict(shared)
        m.update(_core_inputs(inputs, c))
        in_maps.append(m)
    res = run_bass_kernel_spmd(nc, in_maps, core_ids=list(range(NCORES)))
    outs = [np.asarray(r["out"]).reshape(NSEQ, SEQ, D) for r in res.results]
    return np.concatenate(outs, 0).astype(np.float32)
```
